# Optimizing a Trainium2 kernel written in Bass

```python
import jax, jax.numpy as jnp
from jax import lax
import numpy as np

D_MODEL = 1024
BATCH = 16
SEQ = 2048
DEPTH = 4

N_META = 16
HEAD_DIM = 64
A_Q_HEADS = D_MODEL // HEAD_DIM
A_KV_HEADS = A_Q_HEADS // 4
IDX_HEADS = 8
IDX_DIM = 64
B_HEADS = D_MODEL // HEAD_DIM
D_FF = 4 * D_MODEL
BLOCK = 128
TOPK_MAX = 256
ROPE_THETA = 10000.0
EPS = 1e-6
N_A = DEPTH // 2
N_B = DEPTH - N_A

A_Q_W = A_Q_HEADS * HEAD_DIM
A_KV_W = A_KV_HEADS * HEAD_DIM
IDX_Q_W = IDX_HEADS * IDX_DIM
A_SPLITS = (A_Q_W, A_Q_W + A_KV_W, A_Q_W + 2 * A_KV_W,
            A_Q_W + 2 * A_KV_W + IDX_Q_W, A_Q_W + 2 * A_KV_W + IDX_Q_W + IDX_DIM)
A_IN_W = A_SPLITS[-1] + IDX_HEADS
B_W = B_HEADS * HEAD_DIM
KV_IN_W = 2 * B_W + B_HEADS

kernel_name = "yoco_dsa_fox_hybrid"


def rms_norm(x, g):
    xf = x.astype(jnp.float32)
    y = xf * lax.rsqrt(jnp.mean(xf * xf, axis=-1, keepdims=True) + EPS)
    return (y * g.astype(jnp.float32)).astype(x.dtype)


def rope_tables(T, dim):
    inv = 1.0 / (ROPE_THETA ** (jnp.arange(0, dim, 2, dtype=jnp.float32) / dim))
    ang = jnp.arange(T, dtype=jnp.float32)[:, None] * inv[None, :]
    return jnp.cos(ang), jnp.sin(ang)


def rope(x, cos, sin):
    x1, x2 = jnp.split(x, 2, axis=-1)
    c = cos[None, :, None, :]
    s = sin[None, :, None, :]
    return jnp.concatenate([x1 * c - x2 * s, x1 * s + x2 * c], axis=-1).astype(x.dtype)


def sq_relu_mlp(x, w1, w2):
    h = jax.nn.relu(x @ w1)
    return (h * h) @ w2


def dsa_attention(h, w_in, q_gain, k_gain, w_out, cos, sin, topk):
    B, T, _ = h.shape
    R = A_Q_HEADS // A_KV_HEADS
    nblk = T // BLOCK
    q, k, v, qi, ki, wi = jnp.split(h @ w_in, list(A_SPLITS), axis=-1)
    q = rope(rms_norm(q.reshape(B, T, A_Q_HEADS, HEAD_DIM), q_gain), cos, sin)
    k = rope(rms_norm(k.reshape(B, T, A_KV_HEADS, HEAD_DIM), k_gain), cos, sin)
    v = v.reshape(B, T, A_KV_HEADS, HEAD_DIM)
    qi = rope(qi.reshape(B, T, IDX_HEADS, IDX_DIM), cos, sin)
    ki = rope(ki[:, :, None, :], cos, sin)[:, :, 0, :]
    wi = wi.astype(jnp.float32) * (IDX_HEADS ** -0.5 * IDX_DIM ** -0.5)
    qb = q.reshape(B, nblk, BLOCK, A_KV_HEADS, R, HEAD_DIM)
    qib = qi.reshape(B, nblk, BLOCK, IDX_HEADS, IDX_DIM)
    wib = wi.reshape(B, nblk, BLOCK, IDX_HEADS)
    key_pos = jnp.arange(T)
    scale = HEAD_DIM ** -0.5

    def per_seq(args):
        qb_s, qib_s, wib_s, k_s, v_s, ki_s = args

        def per_block(bargs):
            blk, q_blk, qi_blk, w_blk = bargs
            qpos = blk * BLOCK + jnp.arange(BLOCK)
            causal = key_pos[None, :] <= qpos[:, None]
            logits = jnp.einsum('qhd,sd->qhs', qi_blk, ki_s,
                                preferred_element_type=jnp.float32)
            score = jnp.einsum('qh,qhs->qs', w_blk, jax.nn.relu(logits))
            score = jnp.where(causal, score, -jnp.inf)
            _, idx = lax.top_k(score, topk)
            valid = idx <= qpos[:, None]
            k_sel = k_s[idx]
            v_sel = v_s[idx]
            s = jnp.einsum('qgrd,qkgd->qgrk', q_blk, k_sel,
                           preferred_element_type=jnp.float32) * scale
            s = jnp.where(valid[:, None, None, :], s, -jnp.inf)
            p = jax.nn.softmax(s, axis=-1).astype(v_s.dtype)
            return jnp.einsum('qgrk,qkgd->qgrd', p, v_sel)

        return lax.map(per_block, (jnp.arange(nblk), qb_s, qib_s, wib_s))

    o = lax.map(per_seq, (qb, qib, wib, k, v, ki))
    return o.reshape(B, T, A_Q_W) @ w_out


def shared_kv(h, w_kv, f_bias, k_gain):
    B, T, _ = h.shape
    k, v, fl = jnp.split(h @ w_kv, [B_W, 2 * B_W], axis=-1)
    k = rms_norm(k.reshape(B, T, B_HEADS, HEAD_DIM), k_gain)
    v = v.reshape(B, T, B_HEADS, HEAD_DIM)
    log_f = jax.nn.log_sigmoid(fl.astype(jnp.float32) + f_bias.astype(jnp.float32))
    c = jnp.cumsum(log_f, axis=1).transpose(0, 2, 1)
    return k, v, c


def fox_attention(h, w_q, q_gain, w_out, k, v, c):
    B, T, _ = h.shape
    q = rms_norm((h @ w_q).reshape(B, T, B_HEADS, HEAD_DIM), q_gain)
    scale = HEAD_DIM ** -0.5
    outs = []
    for blk in range(T // BLOCK):
        q0, q1 = blk * BLOCK, (blk + 1) * BLOCK
        s = jnp.einsum('bqhd,bshd->bhqs', q[:, q0:q1], k[:, :q1],
                       preferred_element_type=jnp.float32) * scale
        decay = c[:, :, q0:q1, None] - c[:, :, None, :q1]
        causal = jnp.arange(q0, q1)[:, None] >= jnp.arange(q1)[None, :]
        s = jnp.where(causal, s + decay, -jnp.inf)
        p = jax.nn.softmax(s, axis=-1).astype(v.dtype)
        outs.append(jnp.einsum('bhqs,bshd->bqhd', p, v[:, :q1]))
    o = jnp.concatenate(outs, axis=1)
    return o.reshape(B, T, B_W) @ w_out


def setup_inputs(seed: int = 0) -> dict:
    key = jax.random.key(seed)
    ks = jax.random.split(key, 18)

    def dense(k, shape, fan_in, scale=1.0):
        return jax.random.normal(k, shape, jnp.float32) * (scale * fan_in ** -0.5)

    def gain(k, shape):
        return 1.0 + 0.02 * jax.random.normal(k, shape, jnp.float32)

    kv_kv = dense(ks[11], (D_MODEL, 2 * B_W), D_MODEL)
    kv_f = dense(ks[17], (D_MODEL, B_HEADS), D_MODEL, 0.25)
    return {
        "x": jax.random.normal(ks[0], (BATCH, SEQ, D_MODEL), jnp.float32),
        "meta_tokens": jax.random.normal(ks[1], (N_META, D_MODEL), jnp.float32),
        "attn_norm": gain(ks[2], (DEPTH, D_MODEL)),
        "mlp_norm": gain(ks[3], (DEPTH, D_MODEL)),
        "mlp_w1": dense(ks[4], (DEPTH, D_MODEL, D_FF), D_MODEL),
        "mlp_w2": dense(ks[5], (DEPTH, D_FF, D_MODEL), D_FF),
        "a_w_in": dense(ks[6], (N_A, D_MODEL, A_IN_W), D_MODEL),
        "a_q_gain": gain(ks[7], (N_A, HEAD_DIM)),
        "a_k_gain": gain(ks[8], (N_A, HEAD_DIM)),
        "a_w_out": dense(ks[9], (N_A, A_Q_W, D_MODEL), A_Q_W),
        "kv_norm": gain(ks[10], (D_MODEL,)),
        "kv_w": jnp.concatenate([kv_kv, kv_f], axis=1),
        "kv_f_bias": jax.random.uniform(ks[12], (B_HEADS,), jnp.float32, 1.0, 4.0),
        "kv_k_gain": gain(ks[13], (HEAD_DIM,)),
        "b_w_q": dense(ks[14], (N_B, D_MODEL, B_W), D_MODEL),
        "b_q_gain": gain(ks[15], (N_B, HEAD_DIM)),
        "b_w_out": dense(ks[16], (N_B, B_W, D_MODEL), B_W),
    }


def reference(x, meta_tokens, attn_norm, mlp_norm, mlp_w1, mlp_w2,
              a_w_in, a_q_gain, a_k_gain, a_w_out,
              kv_norm, kv_w, kv_f_bias, kv_k_gain,
              b_w_q, b_q_gain, b_w_out):
    B, S, D = x.shape
    T = S + N_META
    Tp = -(-T // BLOCK) * BLOCK
    meta = jnp.broadcast_to(meta_tokens.astype(x.dtype)[None], (B, N_META, D))
    h = jnp.concatenate([meta, x], axis=1)
    h = jnp.pad(h, ((0, 0), (0, Tp - T), (0, 0)))
    cos, sin = rope_tables(Tp, HEAD_DIM)
    topk = min(TOPK_MAX, S // 4)
    shared = None
    for i in range(DEPTH):
        hn = rms_norm(h, attn_norm[i])
        if i < N_A:
            h = h + dsa_attention(hn, a_w_in[i], a_q_gain[i], a_k_gain[i], a_w_out[i],
                                  cos, sin, topk)
        else:
            if i == N_A:
                shared = shared_kv(rms_norm(h, kv_norm), kv_w, kv_f_bias, kv_k_gain)
            j = i - N_A
            h = h + fox_attention(hn, b_w_q[j], b_q_gain[j], b_w_out[j],
                                  shared[0], shared[1], shared[2])
        h = h + sq_relu_mlp(rms_norm(h, mlp_norm[i]), mlp_w1[i], mlp_w2[i])
    return h[:, N_META:N_META + S]
```

```python
import numpy as np
from contextlib import ExitStack
import concourse.bass as bass
import concourse.mybir as mybir
from concourse.bass_utils import run_bass_kernel_spmd

F32 = mybir.dt.float32
BF16 = mybir.dt.bfloat16
ALU = mybir.AluOpType
AF = mybir.ActivationFunctionType
AX = mybir.AxisListType

D = 1024
S_ = 2048
NMETA = 16
T_ = 2176
NB = 17
DFF = 4096
CH = [(0, 512), (512, 512), (1024, 512), (1536, 512), (2048, 128)]
EPS = 1e-6
NBIS = 16
NEG = -30000.0
IDXSCALE = float(8 ** -0.5 * 64 ** -0.5)
C_COS, C_SIN, C_ID, C_BONES, C_PSW, C_TRIQS, C_TRIT, C_POW, C_ONES = (
    0, T_, 2 * T_, 2 * T_ + 128, 2 * T_ + 256, 2 * T_ + 384, 2 * T_ + 512, 2 * T_ + 640, 2 * T_ + 672)
C_END = C_ONES + 128
G_ATT, G_MLP, G_KVN, G_AQ, G_AK, G_KVK, G_BQ, G_FB, G_END = 0, 32, 64, 72, 76, 80, 81, 83, 84


class Op:
    __slots__ = ("eng", "dma", "sem", "val", "alias")

    def __init__(self, eng, dma=False):
        self.eng = eng
        self.dma = dma
        self.sem = None
        self.val = 0
        self.alias = None


class Res:
    __slots__ = ("ws", "rs")

    def __init__(self):
        self.ws = {}
        self.rs = {}


class Trk:
    def __init__(self, nc, es):
        self.nc = nc
        self.es = es
        self.h = {"pe": nc.tensor, "act": nc.scalar, "dve": nc.vector, "pool": nc.gpsimd, "sp": nc.sync}
        self.esem = {e: es.enter_context(nc.semaphore("S_" + e)) for e in ("pe", "act", "dve", "pool")}
        self.cnt = {e: 0 for e in self.esem}
        self.seen = {e: {} for e in self.h}
        self.pend = {e: [] for e in self.esem}
        self.last = {e: None for e in self.esem}
        self.dmas = []
        self.res = {}
        self.dsem = {}
        self.nwait = 0
        self.nins = 0

    def R(self, key):
        r = self.res.get(key)
        if r is None:
            r = self.res[key] = Res()
        return r

    def _add(self, deps, o, d, kind):
        if (not d.dma) and (not o.dma) and d.eng == o.eng:
            if o.eng == "pe":
                return
        deps.append(d)

    @staticmethod
    def _ispsum(k):
        return isinstance(k, str) and len(k) == 2 and k[0] == "P" and k[1].isdigit()

    def _deps(self, o, r, w):
        pr = [k for k in r if self._ispsum(k)]
        if pr:
            r = [k for k in r if not self._ispsum(k)]
            w = list(w) + [k for k in pr if k not in w]
        deps = []
        key = o.eng if not o.dma else ("dma", id(o))
        for k in r:
            x = self.R(k)
            for d in x.ws.values():
                self._add(deps, o, d, "raw")
        for k in w:
            x = self.R(k)
            for d in x.ws.values():
                self._add(deps, o, d, "waw")
            for d in x.rs.values():
                self._add(deps, o, d, "war")
        for k in r:
            self.R(k).rs[key] = o
        for k in w:
            x = self.R(k)
            if x.rs:
                x.ws = {}
                x.rs = {}
            x.ws[key] = o
        return deps

    def _wait(self, eng, deps):
        sn = self.seen[eng]
        for d in deps:
            if d.alias is not None:
                d = d.alias
            assert d.sem is not None, "dependency on unsignalled op"
            k = id(d.sem)
            if sn.get(k, 0) < d.val:
                self.h[eng].wait_ge(d.sem, d.val)
                sn[k] = d.val
                self.nwait += 1

    def op(self, eng, fn, r=(), w=(), sig=True):
        o = Op(eng)
        deps = self._deps(o, r, w)
        self._wait(eng, deps)
        ins = fn()
        self.nins += 1
        if sig:
            self.cnt[eng] += 1
            o.sem = self.esem[eng]
            o.val = self.cnt[eng]
            ins.then_inc(o.sem, 1)
            for p in self.pend[eng]:
                p.alias = o
            self.pend[eng] = []
        else:
            self.pend[eng].append(o)
        self.last[eng] = o
        return o

    def dma(self, q, sem, pairs, r=(), w=(), extra=(), track=True, **kw):
        S = self.dsem.get(sem)
        if S is None:
            S = self.dsem[sem] = [self.es.enter_context(self.nc.semaphore("D_" + sem)), 0, None]
        o = Op(q, dma=True)
        deps = self._deps(o, r, w) + list(extra)
        if S[2] is not None:
            deps.append(S[2])
        self._wait(q, deps)
        S[1] += 16 * len(pairs)
        o.sem = S[0]
        o.val = S[1]
        for (out, in_) in pairs:
            self.h[q].dma_start(out=out, in_=in_, **kw).then_inc(S[0], 16)
            self.nins += 1
        S[2] = o
        if track:
            self.dmas.append(o)
        return o

    def barrier(self):
        deps = [self.last[e] for e in self.esem if self.last[e] is not None] + self.dmas
        for e in self.h:
            self._wait(e, deps)
        self.dmas = []
        self.res = {}

    def finish(self):
        self._wait("sp", list(self.dmas))


class _Stop(Exception):
    pass


def build(n_layers=4, dbg=False, n_seq=2, stop=None):
    nc = bass.Bass("TRN2", target_bir_lowering=False)

    def din(name, shape, dt=F32):
        return nc.dram_tensor(name, shape, dt, kind="ExternalInput").ap()

    def dint(name, shape, dt):
        return nc.dram_tensor(name, shape, dt, kind="Internal").ap()

    x_d = din("x", [2, S_, D])
    meta_d = din("meta", [NMETA, D])
    w1_d = din("mlp_w1", [4, D, DFF])
    w2_d = din("mlp_w2", [4, DFF, D])
    win_d = din("a_w_in", [2, D, 2120])
    wout_d = din("a_w_out", [2, D, D])
    kvw_d = din("kv_w", [D, 2064])
    bwq_d = din("b_w_q", [2, D, D])
    bwout_d = din("b_w_out", [2, D, D])
    cst_d = din("cst", [128, C_END])
    gv_d = din("gv", [128, G_END])
    out_d = nc.dram_tensor("out", [2, S_, D], F32, kind="ExternalOutput").ap()
    dbg_d = None
    if dbg:
        dbg_d = nc.dram_tensor("dbg", [8, 128, T_], F32, kind="ExternalOutput").ap()

    w1b = dint("w1b", [4, D, DFF], BF16)
    w2b = dint("w2b", [4, DFF, D], BF16)
    winb = dint("winb", [2, D, 2120], BF16)
    woutb = dint("woutb", [2, D, D], BF16)
    kvwb = dint("kvwb", [D, 2064], BF16)
    bwqb = dint("bwqb", [2, D, D], BF16)
    bwoutb = dint("bwoutb", [2, D, D], BF16)
    hT_d = dint("hT_d", [8, 128, T_], F32)
    qT_d = dint("qT_d", [8, 128, T_], BF16)
    qiT_d = dint("qiT_d", [4, 128, T_], BF16)
    qaug_d = dint("qaug_d", [16, 70, T_], BF16)
    KT_d = dint("KT_d", [16, 70, T_], BF16)
    Va_d = dint("Va_d", [16, 128, NB, 128], BF16)

    es = ExitStack()
    with es:
        T = Trk(nc, es)

        uniq = [0]

        done = [False]

        def chk(k):
            if stop == k:
                T.barrier()
                done[0] = True
            return done[0]

        def sb(st, name, shape, dt):
            uniq[0] += 1
            return st.enter_context(nc.sbuf_tensor(f"{name}_u{uniq[0]}", shape, dt))

        XT = sb(es, "XT", [128, 8, T_], BF16)
        WR = [sb(es, f"WR{i}", [128, 4096], BF16) for i in range(4)]
        cstf = sb(es, "cstf", [128, C_END - C_ID], F32)
        cstb = sb(es, "cstb", [128, 6 * 128], BF16)
        gv = sb(es, "gv", [128, G_END], F32)
        gq8 = sb(es, "gq8", [128, 2], F32)
        nfb = sb(es, "nfb", [128, 1], F32)
        epsb = sb(es, "epsb", [128, 1], F32)
        thrneg = sb(es, "thrneg", [128, 1], F32)
        PS = [es.enter_context(nc.psum_tensor(f"PS{i}", [128, 512], F32)) for i in range(8)]

        identf = cstf[:, 0:128]
        pow2 = cstf[:, C_POW - C_ID:C_POW - C_ID + 32]
        identb = cstb[:, 0:128]
        bonesb = cstb[:, 128:256]
        pswb = cstb[:, 256:384]
        triqsb = cstb[:, 384:512]
        triTb = cstb[:, 512:640]
        onesb = cstb[:, 640:768]

        wr_i = [0]

        wconv_ops = {}

        def wload(pairs_fn, wkey):
            i = wr_i[0] % len(WR)
            wr_i[0] += 1
            T.dma("sp", f"wr{i}", pairs_fn(WR[i]), w=(("WR", i),), extra=(wconv_ops[wkey],))
            return i

        def mm(out, lhsT, rhs, start, stop, r, w, sig=None):
            if sig is None:
                sig = stop
            return T.op("pe", lambda: nc.tensor.matmul(out, lhsT=lhsT, rhs=rhs, start=start, stop=stop), r, w, sig)

        def act(out, in_, func, r, w, **kw):
            return T.op("act", lambda: nc.scalar.activation(out=out, in_=in_, func=func, **kw), r, w)

        def ts(eng, out, in0, s1, s2, op0, op1, r, w, **kw):
            h = nc.vector if eng == "dve" else nc.gpsimd
            if op1 is None:
                return T.op(eng, lambda: h.tensor_scalar(out=out, in0=in0, scalar1=s1, scalar2=None, op0=op0, **kw), r, w)
            return T.op(eng, lambda: h.tensor_scalar(out=out, in0=in0, scalar1=s1, scalar2=s2, op0=op0, op1=op1, **kw), r, w)

        def tt(eng, out, in0, in1, op, r, w):
            h = nc.vector if eng == "dve" else nc.gpsimd
            return T.op(eng, lambda: h.tensor_tensor(out=out, in0=in0, in1=in1, op=op), r, w)

        def stt(out, in0, scalar, in1, op0, op1, r, w):
            return T.op("dve", lambda: nc.vector.scalar_tensor_tensor(out=out, in0=in0, scalar=scalar, in1=in1,
                                                                      op0=op0, op1=op1), r, w)

        def recip(out, in_, r, w):
            return T.op("dve", lambda: nc.vector.reciprocal(out=out, in_=in_), r, w)

        def cp(eng, out, in_, r, w):
            if eng == "act":
                return T.op("act", lambda: nc.scalar.copy(out=out, in_=in_), r, w)
            h = nc.vector if eng == "dve" else nc.gpsimd
            return T.op(eng, lambda: h.tensor_copy(out=out, in_=in_), r, w)

        def k8(ap, k=8):
            return ap.rearrange("p (k n) -> p k n", k=k)

        T.dma("sp", "cst", [(cstf[:], cst_d[:, C_ID:C_END]), (gv[:], gv_d[:, :])], w=("cstf", "gv"))
        conv = [("win0", winb[0], win_d[0]), ("wout0", woutb[0], wout_d[0]), ("w1_0", w1b[0], w1_d[0]), ("w2_0", w2b[0], w2_d[0]),
                ("win1", winb[1], win_d[1]), ("wout1", woutb[1], wout_d[1]), ("w1_1", w1b[1], w1_d[1]), ("w2_1", w2b[1], w2_d[1]),
                ("kvw", kvwb, kvw_d), ("bwq0", bwqb[0], bwq_d[0]), ("bwout0", bwoutb[0], bwout_d[0]),
                ("w1_2", w1b[2], w1_d[2]), ("w2_2", w2b[2], w2_d[2]), ("bwq1", bwqb[1], bwq_d[1]), ("bwout1", bwoutb[1], bwout_d[1]),
                ("w1_3", w1b[3], w1_d[3]), ("w2_3", w2b[3], w2_d[3])]
        cp("dve", cstb[:, 0:640], cstf[:, 0:640], r=("cstf",), w=("cstb",))
        cp("dve", cstb[:, 640:768], cstf[:, C_ONES - C_ID:C_ONES - C_ID + 128], r=("cstf",), w=("cstb",))
        ts("dve", gq8[:], gv[:, G_BQ:G_BQ + 2], 0.125, None, ALU.mult, None, r=("gv",), w=("gq8",))
        ts("dve", nfb[:], gv[:, G_FB:G_FB + 1], -1.0, None, ALU.mult, None, r=("gv",), w=("nfb",))
        T.op("pool", lambda: nc.gpsimd.memset(epsb[:], EPS), w=("epsb",))
        T.op("pool", lambda: nc.gpsimd.memset(thrneg[:], -1e29), w=("thrneg",))
        T.barrier()
        for (wk, dst_, src_) in conv:
            wconv_ops[wk] = T.dma("pool", "wc_" + wk, [(dst_, src_)], track=(wk in ("win0", "wout0", "w1_0", "w2_0")))

        def norm_cols(src_fn, src_res, gcol, c0, n, tmp):
            sq, std, rstd = tmp
            pb = PS[7]
            for kc in range(8):
                act(sq[:, kc, 0:n], src_fn(kc), AF.Square, r=src_res, w=(("nsq", kc),))
            for kc in range(8):
                mm(pb[:, 0:n], onesb, sq[:, kc, 0:n], kc == 0, kc == 7, r=(("nsq", kc),), w=("P7",))
            act(std[:, 0:n], pb[:, 0:n], AF.Ln, r=("P7",), w=("nstd",), bias=epsb[:, 0:1], scale=1.0 / D)
            act(rstd[:, 0:n], std[:, 0:n], AF.Exp, r=("nstd",), w=("nrstd",), scale=-0.5)
            for kc in range(8):
                stt(XT[:, kc, c0:c0 + n], src_fn(kc), gv[:, gcol + kc:gcol + kc + 1], rstd[:, 0:n], ALU.mult, ALU.mult,
                    r=tuple(src_res) + ("nrstd",), w=(("XT", kc, c0),))

        def norm_from_dram(gcol):
            with ExitStack() as ph:
                hc = [sb(ph, f"n_hc{i}", [128, 8, 512], F32) for i in range(2)]
                sq = sb(ph, "n_sq", [128, 8, 512], BF16)
                std = sb(ph, "n_std", [128, 512], F32)
                rstd = sb(ph, "n_rstd", [128, 512], F32)
                for ci, (c0, n) in enumerate(CH):
                    b = hc[ci % 2]
                    T.dma("sp", f"nhc{ci % 2}", [(b[:, :, 0:n], hT_d[:, :, c0:c0 + n].rearrange("k p n -> p k n"))],
                          w=(("nhc", ci % 2),))
                    norm_cols(lambda kc, b=b, n=n: b[:, kc, 0:n], (("nhc", ci % 2),), gcol, c0, n, (sq, std, rstd))
                T.barrier()

        def input_phase(s):
            with ExitStack() as ph:
                xin = [sb(ph, f"i_x{i}", [128, D], F32) for i in range(2)]
                stg = [sb(ph, f"i_s{i}", [128, 8, 512], F32) for i in range(2)]
                for ci, (c0, n) in enumerate(CH):
                    st = stg[ci % 2]
                    for bb in range(n // 128):
                        b = c0 // 128 + bb
                        xb = xin[b % 2]
                        rk = ("ix", b % 2)
                        if b == 0:
                            T.dma("sp", f"ix{b % 2}", [(xb[0:16, :], meta_d[:, :]), (xb[16:128, :], x_d[s, 0:112, :])], w=(rk,))
                        elif b < 16:
                            T.dma("sp", f"ix{b % 2}", [(xb[:, :], x_d[s, 128 * b - 16:128 * b + 112, :])], w=(rk,))
                        else:
                            T.op("pool", lambda xb=xb: nc.gpsimd.memset(xb[:, :], 0.0), w=(rk,))
                            T.dma("sp", f"ix{b % 2}", [(xb[0:16, :], x_d[s, 2032:2048, :])], w=(rk,))
                        for half in range(2):
                            pidx = (2 * b + half) % 4
                            pb = PS[pidx]
                            pk = f"P{pidx}"
                            for j in range(4):
                                kc = half * 4 + j
                                T.op("pe", lambda pb=pb, j=j, kc=kc, xb=xb: nc.tensor.transpose(
                                    out=pb[:, j * 128:(j + 1) * 128], in_=xb[:, kc * 128:(kc + 1) * 128], identity=identf),
                                    r=(rk,), w=(pk,), sig=(j == 3))
                            eng = "act" if half == 0 else "dve"
                            cp(eng, st[:, half * 4:half * 4 + 4, bb * 128:(bb + 1) * 128],
                               k8(pb[:, :], 4), r=(pk,), w=(("ist", ci % 2),))
                    T.dma("sp", f"ist{ci % 2}", [(hT_d[:, :, c0:c0 + n].rearrange("k p n -> p k n"), st[:, :, 0:n])],
                          r=(("ist", ci % 2),))
                T.barrier()

        def feat_proj(wsrc, colbase, ntiles, Mt, epi, wkey):
            wcols = ntiles * Mt
            i = wload(lambda s_: [(k8(s_[:, 0:8 * wcols]), wsrc[:, :, colbase:colbase + wcols])], wkey)
            wt = k8(WR[i][:, 0:8 * wcols])
            pending = None
            for t_ in range(ntiles):
                for ci, (c0, n) in enumerate(CH):
                    pidx = (t_ * 5 + ci) % 4
                    pb = PS[pidx]
                    pk = f"P{pidx}"
                    for kc in range(8):
                        mm(pb[0:Mt, 0:n], wt[:, kc, t_ * Mt:(t_ + 1) * Mt], XT[:, kc, c0:c0 + n], kc == 0, kc == 7,
                           r=(("WR", i), ("XT", kc, c0)), w=(pk,))
                    if pending is not None:
                        epi(*pending)
                    pending = (t_, ci, c0, n, pb, pk)
            if pending is not None:
                epi(*pending)

        def alloc_epi(ph):
            d = {}
            d["sqb"] = sb(ph, "e_sqb", [128, 512], BF16)
            d["qb"] = sb(ph, "e_qb", [128, 512], BF16)
            d["std"] = sb(ph, "e_std", [128, 512], F32)
            d["rstd"] = sb(ph, "e_rstd", [128, 512], F32)
            d["t1"] = sb(ph, "e_t1", [128, 512], F32)
            d["t2"] = sb(ph, "e_t2", [128, 512], F32)
            d["o"] = [sb(ph, f"e_o{i}", [128, 512], BF16) for i in range(2)]
            d["oi"] = 0
            return d

        def head_rstd(E, pb, pk, n, P=128):
            act(E["sqb"][0:P, 0:n], pb[0:P, 0:n], AF.Square, r=(pk,), w=("e_sq",))
            mm(PS[6][0:P, 0:n], bonesb[0:P, 0:P], E["sqb"][0:P, 0:n], True, True, r=("e_sq",), w=("P6",))
            act(E["std"][0:P, 0:n], PS[6][0:P, 0:n], AF.Ln, r=("P6",), w=("e_std",), bias=epsb[0:P, 0:1], scale=1.0 / 64)
            act(E["rstd"][0:P, 0:n], E["std"][0:P, 0:n], AF.Exp, r=("e_std",), w=("e_rstd",), scale=-0.5)

        def rope_epi(E, pb, pk, n, c0, P, gcol, has_norm, outs, wres, cosT, sinT):
            cp("act", E["qb"][0:P, 0:n], pb[0:P, 0:n], r=(pk,), w=("e_qb",))
            mm(PS[5][0:P, 0:n], pswb[0:P, 0:P], E["qb"][0:P, 0:n], True, True, r=("e_qb",), w=("P5",))
            t1, t2 = E["t1"], E["t2"]
            if has_norm:
                head_rstd(E, pb, pk, n, P)
                stt(t1[0:P, 0:n], pb[0:P, 0:n], gv[0:P, gcol:gcol + 1], cosT[0:P, c0:c0 + n], ALU.mult, ALU.mult,
                    r=(pk, "cos"), w=("e_t1",))
                stt(t2[0:P, 0:n], PS[5][0:P, 0:n], gv[0:P, gcol + 1:gcol + 2], sinT[0:P, c0:c0 + n], ALU.mult, ALU.mult,
                    r=("P5", "cos"), w=("e_t2",))
                tt("pool", t1[0:P, 0:n], t1[0:P, 0:n], t2[0:P, 0:n], ALU.add, r=("e_t1", "e_t2"), w=("e_t1",))
                for (o_ap, sl) in outs:
                    tt("dve", o_ap, t1[sl, 0:n], E["rstd"][sl, 0:n], ALU.mult, r=("e_t1", "e_rstd"), w=wres)
            else:
                stt(t1[0:P, 0:n], pb[0:P, 0:n], 1.0, cosT[0:P, c0:c0 + n], ALU.mult, ALU.mult, r=(pk, "cos"), w=("e_t1",))
                stt(t2[0:P, 0:n], PS[5][0:P, 0:n], 1.0, sinT[0:P, c0:c0 + n], ALU.mult, ALU.mult, r=("P5", "cos"), w=("e_t2",))
                for (o_ap, sl) in outs:
                    tt("dve", o_ap, t1[sl, 0:n], t2[sl, 0:n], ALU.add, r=("e_t1", "e_t2"), w=wres)

        def headnorm_store_epi(E, gain_ap, dst_d, tbase):
            def epi(t_, ci, c0, n, pb, pk):
                head_rstd(E, pb, pk, n)
                oi = E["oi"] % 2
                E["oi"] += 1
                o = E["o"][oi]
                stt(o[:, 0:n], pb[:, 0:n], gain_ap, E["rstd"][:, 0:n], ALU.mult, ALU.mult, r=(pk, "e_rstd"), w=(("e_o", oi),))
                hd = 2 * (tbase + t_)
                T.dma("sp", f"eo{oi}", [(dst_d[hd, 0:64, c0:c0 + n], o[0:64, 0:n]),
                                          (dst_d[hd + 1, 0:64, c0:c0 + n], o[64:128, 0:n])], r=(("e_o", oi),))
            return epi

        def run_pipe(steps, L):
            st = {}
            n = len(steps)
            for k in range(n + L):
                if k < n:
                    st[k] = steps[k][0]()
                if k - L >= 0:
                    steps[k - L][1](st.pop(k - L))

        def attn_A(KTd, Vau, kiTd, absw, sgn):
            with ExitStack() as ph:
                qTc = [sb(ph, f"a_qTc{i}", [128, 8, 512], BF16) for i in range(2)]
                qiTc = [sb(ph, f"a_qiTc{i}", [128, 4, 512], BF16) for i in range(2)]
                score = [sb(ph, f"a_score{i}", [128, T_], F32) for i in range(2)]
                mb = [sb(ph, f"a_mb{i}", [128, T_], BF16) for i in range(2)]
                mbT = [sb(ph, f"a_mbT{i}", [128, NB, 128], BF16) for i in range(2)]
                Rb = [sb(ph, f"a_Rb{i}", [128, 512], BF16) for i in range(3)]
                PT = [sb(ph, f"a_PT{i}", [128, 512], BF16) for i in range(4)]
                Dm = [sb(ph, f"a_Dm{i}", [128, 8, 128], BF16) for i in range(2)]
                rden = [sb(ph, f"a_rden{i}", [128, 512], F32) for i in range(2)]
                sm = [sb(ph, f"a_sm{i}", [128, 8], F32) for i in range(2)]
                dks = [sb(ph, f"a_dks{i}", [128, 32], F32) for i in range(2)]
                TRb = PS[7][:, :].bitcast(BF16)
                SB = [0, 1, 5]
                loaded = set()

                def load_chunk(c):
                    if c in loaded:
                        return
                    loaded.add(c)
                    cc0, cn = CH[c]
                    T.dma("sp", f"qc{c % 2}",
                          [(qTc[c % 2][:, :, 0:cn], qT_d[:, :, cc0:cc0 + cn].rearrange("k p n -> p k n")),
                           (qiTc[c % 2][:, :, 0:cn], qiT_d[:, :, cc0:cc0 + cn].rearrange("k p n -> p k n"))], w=(("qc", c % 2),))

                def idx_gen(i):
                    p = i % 2
                    c = i // 4
                    qoff = (i % 4) * 128
                    qk = ("qc", c % 2)
                    load_chunk(c)
                    qiT = qiTc[c % 2]
                    for h in range(8):
                        ts("pool", Dm[p][:, h, :], identb, sgn[:, i, h:h + 1], None, ALU.mult, None, w=(("Dm", p, h),), r=())
                    nk = (i + 1) * 128
                    nkc = (nk + 511) // 512
                    for kk in range(nkc):
                        k0 = kk * 512
                        ncol = min(512, nk - k0)
                        last = kk == nkc - 1
                        SC = PS[4]
                        Lp = PS[3]

                        def sc_mm(h):
                            mm(SC[:, 0:ncol], Dm[p][:, h, :], Rb[h % 3][:, 0:ncol], h == 0, (h == 7 and not last),
                               r=(("Dm", p, h), ("Rb", h % 3)), w=("P4",))
                        for h in range(8):
                            if h > 0:
                                sc_mm(h - 1)
                            mm(Lp[:, 0:ncol], qiT[:, h // 2, qoff:qoff + 128], kiTd[:, h % 2, k0:k0 + ncol],
                               True, True, r=(qk,), w=("P3",))
                            act(Rb[h % 3][:, 0:ncol], Lp[:, 0:ncol], AF.Relu, r=("P3",), w=(("Rb", h % 3),),
                                scale=absw[:, i, h:h + 1])
                            yield
                        sc_mm(7)
                        if last:
                            dc = nk - 128 - k0
                            mm(SC[:, dc:dc + 128], identb, triqsb, False, True, r=(), w=("P4",))
                        cp("dve", score[p][:, k0:k0 + ncol], SC[:, 0:ncol], r=("P4",), w=(("score", p),))
                        yield

                def bis_gen(i):
                    p = i % 2
                    nk = (i + 1) * 128
                    hi, lo, dd, mid, cnt, tq = (sm[p][:, k:k + 1] for k in range(6))
                    if i >= 2:
                        T.op("dve", lambda: nc.vector.tensor_reduce(out=hi, in_=score[p][:, 0:nk], axis=AX.X, op=ALU.max),
                             r=(("score", p),), w=(("hi", p),))
                        T.op("dve", lambda: nc.vector.tensor_reduce(out=lo, in_=score[p][:, 0:256], axis=AX.X, op=ALU.min),
                             r=(("score", p),), w=(("lo", p),))
                        tt("dve", dd, hi, lo, ALU.subtract, r=(("hi", p), ("lo", p)), w=(("dd", p),))
                        ts("dve", dks[p][:, 0:NBIS], pow2[:, 0:NBIS], dd, None, ALU.mult, None, r=(("dd", p),), w=(("dks", p),))
                        yield
                        for k in range(NBIS):
                            tt("dve", mid, lo, dks[p][:, k:k + 1], ALU.add, r=(("lo", p), ("dks", p)), w=(("mid", p),))
                            ts("dve", mb[p][:, 0:nk], score[p][:, 0:nk], mid, None, ALU.is_ge, ALU.add,
                               r=(("score", p), ("mid", p)), w=(("mb", p), ("cnt", p)), accum_out=cnt)
                            stt(tq, cnt, 255.5, dks[p][:, k:k + 1], ALU.is_ge, ALU.mult, r=(("cnt", p), ("dks", p)), w=(("tq", p),))
                            tt("dve", lo, lo, tq, ALU.add, r=(("lo", p), ("tq", p)), w=(("lo", p),))
                            yield
                        thr = lo
                    else:
                        thr = thrneg[:, 0:1]
                    ts("dve", mb[p][:, 0:nk], score[p][:, 0:nk], thr, NEG, ALU.is_lt, ALU.mult, r=(("score", p), ("lo", p)),
                       w=(("mb", p),))
                    yield
                    for j0 in range(0, i + 1, 4):
                        nj = min(4, i + 1 - j0)
                        for jj in range(nj):
                            T.op("pe", lambda jj=jj, j0=j0: nc.tensor.transpose(
                                out=TRb[:, jj * 128:(jj + 1) * 128], in_=mb[p][:, (j0 + jj) * 128:(j0 + jj + 1) * 128],
                                identity=identb), r=(("mb", p),), w=("P7",), sig=(jj == nj - 1))
                        cp("dve", mbT[p][:, j0:j0 + nj, :], k8(TRb[:, 0:nj * 128], nj), r=("P7",), w=(("mbT", p),))
                        yield

                def n_idx(i):
                    return 9 * (((i + 1) * 128 + 511) // 512)

                def n_bis(i):
                    return (NBIS + 1 if i >= 2 else 0) + 1 + (i + 4) // 4

                def drain(gen):
                    if gen is not None:
                        for _ in gen:
                            pass

                drain(idx_gen(0))
                drain(bis_gen(0))
                drain(idx_gen(1))
                scn = [0]
                for i in range(NB):
                    p = i % 2
                    c = i // 4
                    cc0, cn = CH[c]
                    qoff = (i % 4) * 128
                    qk = ("qc", c % 2)
                    qT = qTc[c % 2]
                    gA = idx_gen(i + 2) if i + 2 < NB else None
                    gB = bis_gen(i + 1) if i + 1 < NB else None
                    nA = n_idx(i + 2) if gA is not None else 0
                    nB = n_bis(i + 1) if gB is not None else 0
                    nst = 4 * (i + 1)
                    mcount = [0]

                    def weave():
                        m = mcount[0]
                        mcount[0] += 1
                        for (gen, ntot) in ((gB, nB), (gA, nA)):
                            if gen is None:
                                continue
                            k = (ntot * (m + 1) + nst - 1) // nst - (ntot * m + nst - 1) // nst
                            for _ in range(k):
                                try:
                                    next(gen)
                                except StopIteration:
                                    break
                    steps = []
                    for g in range(4):
                        O = PS[6 if g % 2 == 0 else 2]
                        ok = "P6" if g % 2 == 0 else "P2"
                        for j in range(i + 1):
                            def s_fn(g=g, j=j):
                                k_ = scn[0]
                                scn[0] += 1
                                sbk = SB[k_ % 3]
                                Sp = PS[sbk]
                                sk = f"P{sbk}"
                                pt = PT[k_ % 4]
                                ptk = ("PT", k_ % 4)
                                js = slice(j * 128, (j + 1) * 128)
                                weave()
                                mm(k8(Sp[:, 0:512], 4), identb, mbT[p][:, j:j + 1, :].broadcast_to([128, 4, 128]), True, False,
                                   r=(("mbT", p),), w=(sk,))
                                mm(k8(Sp[:, 0:256], 2), KTd[:, 0, g, js], qT[:, 2 * g:2 * g + 2, qoff:qoff + 128], False, False,
                                   r=(qk,), w=(sk,))
                                mm(k8(Sp[:, 256:512], 2), KTd[:, 1, g, js], qT[:, 2 * g:2 * g + 2, qoff:qoff + 128], False, True,
                                   r=(qk,), w=(sk,))
                                act(pt[:, :], Sp[:, :], AF.Exp, r=(sk,), w=(ptk,), scale=0.125)
                                return pt, ptk

                            def v_fn(st, g=g, j=j, O=O, ok=ok):
                                pt, ptk = st
                                mm(O[:, :], Vau[:, j, g, :], pt[:, :], j == 0, j == i, r=(ptk,), w=(ok,))
                                if j == i:
                                    rd = rden[g % 2]
                                    rk = ("rden", g % 2)
                                    act(rd[0:64, :], O[64:128, :], AF.Ln, r=(ok,), w=(rk,))
                                    act(rd[0:64, :], rd[0:64, :], AF.Exp, r=(rk,), w=(rk,), scale=-1.0)
                                    qs = slice(cc0 + qoff, cc0 + qoff + 128)
                                    tt("dve", XT[0:64, 2 * g:2 * g + 2, qs], k8(O[0:64, 0:256], 2), k8(rd[0:64, 0:256], 2), ALU.mult,
                                       r=(ok, rk), w=(("XT", 2 * g, cc0), ("XT", 2 * g + 1, cc0)))
                                    tt("dve", XT[64:128, 2 * g:2 * g + 2, qs], k8(O[0:64, 256:512], 2), k8(rd[0:64, 256:512], 2),
                                       ALU.mult, r=(ok, rk), w=(("XT", 2 * g, cc0), ("XT", 2 * g + 1, cc0)))
                            steps.append((s_fn, v_fn))
                    run_pipe(steps, 2)
                    drain(gB)
                    drain(gA)
                T.barrier()

        def layer_A(l):
            with ExitStack() as pa:
                KTd = sb(pa, "a_KTd", [128, 2, 4, T_], BF16)
                Vau = sb(pa, "a_Vau", [128, NB, 4, 128], BF16)
                kiTd = sb(pa, "a_kiTd", [128, 2, T_], BF16)
                T.op("pool", lambda: nc.gpsimd.memset(KTd[64:128, 0, :, :], 0.0), w=("KTz0",))
                T.op("pool", lambda: nc.gpsimd.memset(KTd[0:64, 1, :, :], 0.0), w=("KTz1",))
                T.op("pool", lambda: nc.gpsimd.memset(kiTd[64:128, 0, :], 0.0), w=("kiz0",))
                T.op("pool", lambda: nc.gpsimd.memset(kiTd[0:64, 1, :], 0.0), w=("kiz1",))
                wi = sb(pa, "a_wi", [128, NB, 8], F32)
                absw = sb(pa, "a_absw", [128, NB, 8], F32)
                sgn = sb(pa, "a_sgn", [128, NB, 8], F32)
                T.op("pool", lambda: nc.gpsimd.memset(Vau[:, :, :, 64:128], 1.0), w=("Vau1",))
                norm_from_dram(G_ATT + 8 * l)
                if chk(2):
                    return
                with ExitStack() as ph:
                    cs = sb(ph, "a_cs", [128, 2 * T_], F32)
                    cosT = cs[:, 0:T_]
                    sinT = cs[:, T_:2 * T_]
                    T.dma("sp", "cs", [(cs[:], cst_d[:, 0:2 * T_])], w=("cos",))
                    E = alloc_epi(ph)
                    wv = winb[l].rearrange("(k p) n -> p k n", p=128)

                    def q_epi(tbase, dst, has_norm, gcol):
                        def epi(t_, ci, c0, n, pb, pk):
                            oi = E["oi"] % 2
                            E["oi"] += 1
                            o = E["o"][oi]
                            rope_epi(E, pb, pk, n, c0, 128, gcol, has_norm, [(o[:, 0:n], slice(0, 128))], (("e_o", oi),), cosT, sinT)
                            T.dma("sp", f"eo{oi}", [(dst[tbase + t_, :, c0:c0 + n], o[:, 0:n])], r=(("e_o", oi),))
                        return epi

                    def k_epi(t_, ci, c0, n, pb, pk):
                        g0, g1 = 2 * t_, 2 * t_ + 1
                        outs = [(KTd[0:64, 0, g0, c0:c0 + n], slice(0, 64)), (KTd[64:128, 1, g0, c0:c0 + n], slice(0, 64)),
                                (KTd[64:128, 1, g1, c0:c0 + n], slice(64, 128)), (KTd[0:64, 0, g1, c0:c0 + n], slice(64, 128))]
                        rope_epi(E, pb, pk, n, c0, 128, G_AK + 2 * l, True, outs, ("KTd",), cosT, sinT)

                    def ki_epi(t_, ci, c0, n, pb, pk):
                        outs = [(kiTd[0:64, 0, c0:c0 + n], slice(0, 64)), (kiTd[64:128, 1, c0:c0 + n], slice(0, 64))]
                        rope_epi(E, pb, pk, n, c0, 64, 0, False, outs, ("kiTd",), cosT, sinT)

                    feat_proj(wv, 0, 4, 128, q_epi(0, qT_d, True, G_AQ + 2 * l), f"win{l}")
                    if chk(21):
                        return
                    feat_proj(wv, 512, 4, 128, q_epi(4, qT_d, True, G_AQ + 2 * l), f"win{l}")
                    feat_proj(wv, 1024, 2, 128, k_epi, f"win{l}")
                    if chk(22):
                        return
                    feat_proj(wv, 1536, 4, 128, q_epi(0, qiT_d, False, 0), f"win{l}")
                    if chk(23):
                        return
                    feat_proj(wv, 2048, 1, 64, ki_epi, f"win{l}")
                    if chk(24):
                        return
                    i = wload(lambda s_: [(k8(s_[:, 0:8 * 264])[:, :, 0:256], wv[:, :, 1280:1536]),
                                          (k8(s_[:, 0:8 * 264])[:, :, 256:264], wv[:, :, 2112:2120])], f"win{l}")
                    wt = k8(WR[i][:, 0:8 * 264])
                    for b in range(NB):
                        pb = PS[b % 4]
                        pk = f"P{b % 4}"
                        c0 = CH[b // 4][0]
                        for kc in range(8):
                            mm(pb[:, 0:264], XT[:, kc, b * 128:(b + 1) * 128], wt[:, kc, :], kc == 0, kc == 7,
                               r=(("WR", i), ("XT", kc, c0)), w=(pk,))
                        cp("act" if b % 2 == 0 else "dve", Vau[:, b, :, 0:64], k8(pb[:, 0:256], 4), r=(pk,), w=("Vau",))
                        cp("dve", wi[:, b, :], pb[:, 256:264], r=(pk,), w=("wi",))
                    act(absw[:], wi[:], AF.Abs, r=("wi",), w=("absw",), scale=IDXSCALE)
                    ts("dve", sgn[:], wi[:], 0.0, 2.0, ALU.is_ge, ALU.mult, r=("wi",), w=("sgn",))
                    ts("dve", sgn[:], sgn[:], -1.0, None, ALU.add, None, r=("sgn",), w=("sgn",))
                    T.barrier()
                if chk(3):
                    return
                attn_A(KTd, Vau, kiTd, absw, sgn)

        def kv_phase2():
            norm_from_dram(G_KVN)
            kvv = kvwb.rearrange("(k p) n -> p k n", p=128)
            with ExitStack() as ph:
                fl = sb(ph, "k_fl", [16, T_], F32)
                with ExitStack() as ph2:
                    E = alloc_epi(ph2)
                    Vst = [sb(ph2, f"k_Vst{i}", [128, 8, 4, 128], BF16) for i in range(2)]
                    for i_ in range(2):
                        T.op("pool", lambda i_=i_: nc.gpsimd.memset(Vst[i_][:, :, :, 64:128], 1.0), w=(("Vst", i_),))
                    feat_proj(kvv, 0, 4, 128, headnorm_store_epi(E, gv[:, G_KVK:G_KVK + 1], KT_d, 0), "kvw")
                    feat_proj(kvv, 512, 4, 128, headnorm_store_epi(E, gv[:, G_KVK:G_KVK + 1], KT_d, 4), "kvw")
                    vi = 0
                    for half in range(2):
                        i = wload(lambda s_: [(k8(s_[:, :]), kvv[:, :, 1024 + 512 * half:1536 + 512 * half])], "kvw")
                        wt = k8(WR[i][:, :])
                        for b in range(NB):
                            pb = PS[b % 4]
                            pk = f"P{b % 4}"
                            ci = b // 4
                            bb = b % 4
                            c0, n = CH[ci]
                            if bb == 0:
                                vi += 1
                            st = Vst[vi % 2]
                            for kc in range(8):
                                mm(pb[:, :], XT[:, kc, b * 128:(b + 1) * 128], wt[:, kc, :], kc == 0, kc == 7,
                                   r=(("WR", i), ("XT", kc, c0)), w=(pk,))
                            cp("act" if b % 2 == 0 else "dve", st[:, :, bb, 0:64], k8(pb[:, :], 8), r=(pk,), w=(("Vst", vi % 2),))
                            if bb == n // 128 - 1:
                                nb_ = n // 128
                                T.dma("sp", f"vst{vi % 2}",
                                      [(Va_d[8 * half + hh, :, 4 * ci:4 * ci + nb_, :], st[:, hh, 0:nb_, :]) for hh in range(8)],
                                      r=(("Vst", vi % 2),))

                    def f_epi(t_, ci, c0, n, pb, pk):
                        cp("dve", fl[0:16, c0:c0 + n], pb[0:16, 0:n], r=(pk,), w=("fl",))
                    feat_proj(kvv, 2048, 1, 16, f_epi, "kvw")
                    T.barrier()
                on = sb(ph, "k_on", [16, T_], F32)
                cc = sb(ph, "k_c", [16, T_], F32)
                r1 = sb(ph, "k_r1", [16, T_], F32)
                qa = sb(ph, "k_qa", [16, 6, T_], BF16)
                ka = sb(ph, "k_ka", [16, 6, T_], BF16)
                T.op("pool", lambda: nc.gpsimd.memset(on[:, :], 1.0), w=("on",))
                T.op("pool", lambda: nc.gpsimd.memset(qa[:, 3:6, :], 1.0), w=("qa1",))
                T.op("pool", lambda: nc.gpsimd.memset(ka[:, 0:3, :], 1.0), w=("ka1",))
                act(r1[:, :], fl[:, :], AF.Exp, r=("fl",), w=("r1",), scale=-1.0, bias=nfb[0:16, 0:1])
                act(fl[:, :], r1[:, :], AF.Ln, r=("r1",), w=("fl",), bias=1.0)
                T.op("dve", lambda: nc.vector.tensor_tensor_scan(out=cc[:, :], data0=on[:, :], data1=fl[:, :], initial=0.0,
                                                                 op0=ALU.mult, op1=ALU.subtract), r=("on", "fl"), w=("cc",))
                cp("dve", qa[:, 0, :], cc[:, :], r=("cc",), w=("qa0",))
                tt("dve", r1[:, :], cc[:, :], qa[:, 0, :], ALU.subtract, r=("cc", "qa0"), w=("r1",))
                cp("dve", qa[:, 1, :], r1[:, :], r=("r1",), w=("qa1b",))
                tt("dve", fl[:, :], r1[:, :], qa[:, 1, :], ALU.subtract, r=("r1", "qa1b"), w=("fl",))
                cp("dve", qa[:, 2, :], fl[:, :], r=("fl",), w=("qa2",))
                ts("dve", ka[:, 3:6, :], qa[:, 0:3, :], -1.0, None, ALU.mult, None, r=("qa0", "qa1b", "qa2"), w=("ka2",))
                T.dma("sp", "caug", [(qaug_d[:, 64:70, :], qa[:, :, :]), (KT_d[:, 64:70, :], ka[:, :, :])],
                      r=("qa0", "qa1b", "qa2", "qa1", "ka1", "ka2"))
                T.barrier()

        def attn_B():
            with ExitStack() as ph:
                slots = [(sb(ph, f"b_qh{i}", [70, T_], BF16), sb(ph, f"b_kh{i}", [70, T_], BF16),
                          sb(ph, f"b_vh{i}", [128, NB, 128], BF16)) for i in range(3)]
                PT = [sb(ph, f"b_PT{i}", [128, 512], BF16) for i in range(6)]
                SBK = [0, 1, 2, 3, 4]
                scn = [0]
                rden = [sb(ph, f"b_rden{i}", [128, 512], F32) for i in range(2)]
                oc = 0
                for h in range(16):
                    sl = h % 3
                    qh, kh, vh = slots[sl]
                    hk = ("bh", sl)
                    T.dma("sp", f"bh{sl}", [(qh[:, :], qaug_d[h]), (kh[:, :], KT_d[h]), (vh[:, :, :], Va_d[h])], w=(hk,))
                    steps = []
                    for ci, (c0, n) in enumerate(CH):
                        O = PS[6 if oc % 2 == 0 else 5]
                        ok = "P6" if oc % 2 == 0 else "P5"
                        rd = rden[oc % 2]
                        rk = ("rden", oc % 2)
                        oc += 1
                        b0 = c0 // 128
                        jlast = b0 + n // 128 - 1
                        for j in range(jlast + 1):
                            rr = max(j - b0, 0)
                            col0 = c0 + rr * 128
                            ncols = c0 + n - col0
                            diag = j >= b0

                            def s_fn(j=j, col0=col0, ncols=ncols, diag=diag):
                                k_ = scn[0]
                                scn[0] += 1
                                Sp = PS[SBK[k_ % 5]]
                                sk = f"P{SBK[k_ % 5]}"
                                pt = PT[k_ % 6]
                                ptk = ("PT", k_ % 6)
                                mm(Sp[:, 0:ncols], kh[0:70, j * 128:(j + 1) * 128], qh[0:70, col0:col0 + ncols], True, not diag,
                                   r=(hk,), w=(sk,))
                                if diag:
                                    mm(Sp[:, 0:128], identb, triTb, False, True, r=(), w=(sk,))
                                act(pt[:, 0:ncols], Sp[:, 0:ncols], AF.Exp, r=(sk,), w=(ptk,))
                                return pt, ptk

                            def v_fn(st, j=j, col0=col0, ncols=ncols, c0=c0, n=n, O=O, ok=ok, rd=rd, rk=rk, jlast=jlast, h=h):
                                pt, ptk = st
                                mm(O[:, col0 - c0:col0 - c0 + ncols], vh[:, j, :], pt[:, 0:ncols], j == 0, j == jlast,
                                   r=(ptk, hk), w=(ok,))
                                if j == jlast:
                                    act(rd[0:64, 0:n], O[64:128, 0:n], AF.Ln, r=(ok,), w=(rk,))
                                    act(rd[0:64, 0:n], rd[0:64, 0:n], AF.Exp, r=(rk,), w=(rk,), scale=-1.0)
                                    pbase = (h % 2) * 64
                                    tt("dve", XT[pbase:pbase + 64, h // 2, c0:c0 + n], O[0:64, 0:n], rd[0:64, 0:n], ALU.mult,
                                       r=(ok, rk), w=(("XT", h // 2, c0),))
                            steps.append((s_fn, v_fn))
                    run_pipe(steps, 3)
                T.barrier()

        def layer_B(j):
            norm_from_dram(G_ATT + 8 * (2 + j))
            with ExitStack() as ph:
                E = alloc_epi(ph)
                wv = bwqb[j].rearrange("(k p) n -> p k n", p=128)
                feat_proj(wv, 0, 4, 128, headnorm_store_epi(E, gq8[:, j:j + 1], qaug_d, 0), f"bwq{j}")
                feat_proj(wv, 512, 4, 128, headnorm_store_epi(E, gq8[:, j:j + 1], qaug_d, 4), f"bwq{j}")
                T.barrier()
            attn_B()

        def outproj_mlp(l, wo_d, s, last, wokey):
            with ExitStack() as ph:
                hTf = sb(ph, "m_hTf", [128, 8, T_], F32)
                for ci, (c0, n) in enumerate(CH):
                    T.dma("sp", "hload", [(hTf[:, :, c0:c0 + n], hT_d[:, :, c0:c0 + n].rearrange("k p n -> p k n"))],
                          w=tuple(("hTf", m, c0) for m in range(8)))
                wov = wo_d.rearrange("(k p) n -> p k n", p=128)

                def o_epi(tbase):
                    def epi(t_, ci, c0, n, pb, pk):
                        m = tbase + t_
                        tt("dve", hTf[:, m, c0:c0 + n], hTf[:, m, c0:c0 + n], pb[:, 0:n], ALU.add, r=(pk, ("hTf", m, c0)),
                           w=(("hTf", m, c0),))
                    return epi
                feat_proj(wov, 0, 4, 128, o_epi(0), wokey)
                feat_proj(wov, 512, 4, 128, o_epi(4), wokey)
                with ExitStack() as ph2:
                    sq = sb(ph2, "n_sq", [128, 8, 512], BF16)
                    std = sb(ph2, "n_std", [128, 512], F32)
                    rstd = sb(ph2, "n_rstd", [128, 512], F32)
                    for ci, (c0, n) in enumerate(CH):
                        norm_cols(lambda kc, c0=c0, n=n: hTf[:, kc, c0:c0 + n], tuple(("hTf", m, c0) for m in range(8)),
                                  G_MLP + 8 * l, c0, n, (sq, std, rstd))
                    T.barrier()
                with ExitStack() as ph2:
                    aT = [sb(ph2, f"m_aT{i}", [128, 4, T_], BF16) for i in range(2)]
                    rt = [sb(ph2, f"m_rt{i}", [128, 512], F32) for i in range(2)]
                    w1v = w1b[l].rearrange("(k p) n -> p k n", p=128)
                    w2v = w2b[l].rearrange("(f p) n -> p f n", p=128)
                    ri = 0
                    pc = 0
                    yc = 0
                    for fg in range(8):
                        i1 = wload(lambda s_: [(k8(s_[:, :]), w1v[:, :, fg * 512:(fg + 1) * 512])], f"w1_{l}")
                        w1t = k8(WR[i1][:, :])
                        a = aT[fg % 2]
                        for f in range(4):
                            for ci, (c0, n) in enumerate(CH):
                                pidx = pc % 4
                                pc += 1
                                pb = PS[pidx]
                                pk = f"P{pidx}"
                                for kc in range(8):
                                    mm(pb[:, 0:n], w1t[:, kc, f * 128:(f + 1) * 128], XT[:, kc, c0:c0 + n], kc == 0, kc == 7,
                                       r=(("WR", i1), ("XT", kc, c0)), w=(pk,))
                                r_ = rt[ri % 2]
                                rk = ("rt", ri % 2)
                                ri += 1
                                act(r_[:, 0:n], pb[:, 0:n], AF.Relu, r=(pk,), w=(rk,))
                                tt("pool", a[:, f, c0:c0 + n], r_[:, 0:n], r_[:, 0:n], ALU.mult, r=(rk,), w=(("aT", fg % 2, f, c0),))
                        i2 = wload(lambda s_: [(k8(s_[:, :], 4), w2v[:, fg * 4:(fg + 1) * 4, :])], f"w2_{l}")
                        w2t = k8(WR[i2][:, :], 4)
                        for m in range(8):
                            for ci, (c0, n) in enumerate(CH):
                                pidx = 4 + yc % 4
                                yc += 1
                                pb = PS[pidx]
                                pk = f"P{pidx}"
                                for f in range(4):
                                    mm(pb[:, 0:n], w2t[:, f, m * 128:(m + 1) * 128], a[:, f, c0:c0 + n], f == 0, f == 3,
                                       r=(("WR", i2), ("aT", fg % 2, f, c0)), w=(pk,))
                                tt("dve", hTf[:, m, c0:c0 + n], hTf[:, m, c0:c0 + n], pb[:, 0:n], ALU.add,
                                   r=(pk, ("hTf", m, c0)), w=(("hTf", m, c0),))
                    T.barrier()
                if not last:
                    for ci, (c0, n) in enumerate(CH):
                        T.dma("sp", "hstore", [(hT_d[:, :, c0:c0 + n].rearrange("k p n -> p k n"), hTf[:, :, c0:c0 + n])])
                    if dbg_d is not None and s == 0:
                        T.dma("sp", "dbgst", [(dbg_d[:, :, :].rearrange("k p n -> p k n"), hTf[:, :, :])])
                else:
                    if dbg_d is not None and s == 0:
                        T.dma("sp", "dbgst", [(dbg_d[:, :, :].rearrange("k p n -> p k n"), hTf[:, :, :])])
                    with ExitStack() as ph2:
                        og = [sb(ph2, f"o_st{i}", [128, D], F32) for i in range(2)]
                        for b in range(NB):
                            o = og[b % 2]
                            okk = ("ost", b % 2)
                            for half in range(2):
                                pidx = (2 * b + half) % 4
                                pb = PS[pidx]
                                pk = f"P{pidx}"
                                for jq in range(4):
                                    kc = half * 4 + jq
                                    T.op("pe", lambda pb=pb, jq=jq, kc=kc, b=b: nc.tensor.transpose(
                                        out=pb[:, jq * 128:(jq + 1) * 128], in_=hTf[:, kc, b * 128:(b + 1) * 128], identity=identf),
                                        r=(), w=(pk,), sig=(jq == 3))
                                cp("act" if half == 0 else "dve", o[:, half * 512:(half + 1) * 512], pb[:, :], r=(pk,), w=(okk,))
                            if b == 0:
                                T.dma("sp", f"ost{b % 2}", [(out_d[s, 0:112, :], o[16:128, :])], r=(okk,))
                            elif b < 16:
                                T.dma("sp", f"ost{b % 2}", [(out_d[s, 128 * b - 16:128 * b + 112, :], o[:, :])], r=(okk,))
                            else:
                                T.dma("sp", f"ost{b % 2}", [(out_d[s, 2032:2048, :], o[0:16, :])], r=(okk,))
                        T.barrier()
                T.barrier()

        chk(0)
        for s in range(n_seq):
            if done[0]:
                break
            input_phase(s)
            if chk(1):
                break
            for l in range(n_layers):
                last = l == n_layers - 1
                if l < 2:
                    layer_A(l)
                    if done[0] or chk(4):
                        break
                    outproj_mlp(l, woutb[l], s, last, f"wout{l}")
                else:
                    if l == 2:
                        kv_phase2()
                    layer_B(l - 2)
                    outproj_mlp(l, bwoutb[l - 2], s, last, f"bwout{l - 2}")
        T.finish()
        stats = (T.nins, T.nwait, dict(T.cnt))
    return nc, stats


def host_consts():
    inv = (1.0 / (np.float32(10000.0) ** (np.arange(0, 64, 2, dtype=np.float32) / np.float32(64)))).astype(np.float32)
    ang = (np.arange(T_, dtype=np.float32)[:, None] * inv[None, :]).astype(np.float32)
    cos = np.cos(ang).astype(np.float32)
    sin = np.sin(ang).astype(np.float32)
    cst = np.zeros((128, C_END), np.float32)
    p = np.arange(128)
    d = p % 64
    cst[:, C_COS:C_COS + T_] = cos[:, d % 32].T
    sg = np.where(d < 32, -1.0, 1.0).astype(np.float32)
    cst[:, C_SIN:C_SIN + T_] = sin[:, d % 32].T * sg[:, None]
    cst[:, C_ID:C_ID + 128] = np.eye(128, dtype=np.float32)
    cst[:, C_BONES:C_BONES + 128] = (p[:, None] // 64 == p[None, :] // 64).astype(np.float32)
    cst[:, C_PSW:C_PSW + 128] = (p[:, None] == (p[None, :] ^ 32)).astype(np.float32)
    cst[:, C_TRIQS:C_TRIQS + 128] = np.where(p[None, :] > p[:, None], -1e30, 0.0).astype(np.float32)
    cst[:, C_TRIT:C_TRIT + 128] = np.where(p[:, None] > p[None, :], NEG, 0.0).astype(np.float32)
    cst[:, C_POW:C_POW + 32] = (0.5 ** np.arange(1, 33, dtype=np.float64)).astype(np.float32)[None, :]
    cst[:, C_ONES:C_ONES + 128] = 1.0
    return cst


def host_gv(attn_norm, mlp_norm, kv_norm, a_q_gain, a_k_gain, kv_k_gain, b_q_gain, kv_f_bias):
    gv = np.zeros((128, G_END), np.float32)
    p = np.arange(128)
    d = p % 64
    for l in range(4):
        gv[:, G_ATT + 8 * l:G_ATT + 8 * l + 8] = np.asarray(attn_norm[l]).reshape(8, 128).T
        gv[:, G_MLP + 8 * l:G_MLP + 8 * l + 8] = np.asarray(mlp_norm[l]).reshape(8, 128).T
    gv[:, G_KVN:G_KVN + 8] = np.asarray(kv_norm).reshape(8, 128).T
    for l in range(2):
        gv[:, G_AQ + 2 * l] = np.asarray(a_q_gain[l])[d]
        gv[:, G_AQ + 2 * l + 1] = np.asarray(a_q_gain[l])[d ^ 32]
        gv[:, G_AK + 2 * l] = np.asarray(a_k_gain[l])[d]
        gv[:, G_AK + 2 * l + 1] = np.asarray(a_k_gain[l])[d ^ 32]
        gv[:, G_BQ + l] = np.asarray(b_q_gain[l])[d]
    gv[:, G_KVK] = np.asarray(kv_k_gain)[d]
    gv[0:16, G_FB] = np.asarray(kv_f_bias)
    return gv


_CACHE = {}


def kernel(x, meta_tokens, attn_norm, mlp_norm, mlp_w1, mlp_w2, a_w_in, a_q_gain, a_k_gain, a_w_out,
           kv_norm, kv_w, kv_f_bias, kv_k_gain, b_w_q, b_q_gain, b_w_out, _n_layers=4, _dbg=False, _n_seq=2, _trace=False, _stop=None, _cores=8):
    f = lambda a: np.ascontiguousarray(np.asarray(a, dtype=np.float32))
    x = f(x)
    key = (_n_layers, _dbg, _n_seq, _stop)
    if key not in _CACHE:
        _CACHE[key] = build(_n_layers, _dbg, _n_seq, _stop)
    nc, stats = _CACHE[key]
    shared = {
        "meta": f(meta_tokens), "mlp_w1": f(mlp_w1), "mlp_w2": f(mlp_w2), "a_w_in": f(a_w_in), "a_w_out": f(a_w_out),
        "kv_w": f(kv_w), "b_w_q": f(b_w_q), "b_w_out": f(b_w_out), "cst": host_consts(),
        "gv": host_gv(f(attn_norm), f(mlp_norm), f(kv_norm), f(a_q_gain), f(a_k_gain), f(kv_k_gain), f(b_q_gain), f(kv_f_bias)),
    }
    in_maps = []
    for c in range(_cores):
        m = dict(shared)
        m["x"] = np.ascontiguousarray(x[2 * c:2 * c + 2])
        in_maps.append(m)
    if _trace:
        res = run_bass_kernel_spmd(nc, in_maps, core_ids=list(range(_cores)), trace=True)
        print('exec_time_ns', res.exec_time_ns)
    else:
        res = run_bass_kernel_spmd(nc, in_maps, core_ids=list(range(_cores)))
    out = np.concatenate([np.asarray(r["out"]) for r in res.results], axis=0).astype(np.float32)
    if _dbg:
        return out, [np.asarray(r["dbg"]) for r in res.results]
    return out
```

```python
import numpy as np
from contextlib import ExitStack
import concourse.bass as bass
import concourse.mybir as mybir
from concourse.bass_utils import run_bass_kernel_spmd

F32 = mybir.dt.float32
BF16 = mybir.dt.bfloat16
ALU = mybir.AluOpType
AF = mybir.ActivationFunctionType
AX = mybir.AxisListType

D = 1024
S_ = 2048
NMETA = 16
T_ = 2176
NB = 17
DFF = 4096
CH = [(0, 512), (512, 512), (1024, 512), (1536, 512), (2048, 128)]
EPS = 1e-6
NBIS = 16
NEG = -30000.0
IDXSCALE = float(8 ** -0.5 * 64 ** -0.5)
C_COS, C_SIN, C_ID, C_BONES, C_PSW, C_TRIQS, C_TRIT, C_POW, C_ONES = (
    0, T_, 2 * T_, 2 * T_ + 128, 2 * T_ + 256, 2 * T_ + 384, 2 * T_ + 512, 2 * T_ + 640, 2 * T_ + 672)
C_END = C_ONES + 128
G_ATT, G_MLP, G_KVN, G_AQ, G_AK, G_KVK, G_BQ, G_FB, G_END = 0, 32, 64, 72, 76, 80, 81, 83, 84


class Op:
    __slots__ = ("eng", "dma", "sem", "val", "alias")

    def __init__(self, eng, dma=False):
        self.eng = eng
        self.dma = dma
        self.sem = None
        self.val = 0
        self.alias = None


class Res:
    __slots__ = ("ws", "rs")

    def __init__(self):
        self.ws = {}
        self.rs = {}


class Trk:
    def __init__(self, nc, es):
        self.nc = nc
        self.es = es
        self.h = {"pe": nc.tensor, "act": nc.scalar, "dve": nc.vector, "pool": nc.gpsimd, "sp": nc.sync}
        self.esem = {e: es.enter_context(nc.semaphore("S_" + e)) for e in ("pe", "act", "dve", "pool")}
        self.cnt = {e: 0 for e in self.esem}
        self.seen = {e: {} for e in self.h}
        self.pend = {e: [] for e in self.esem}
        self.last = {e: None for e in self.esem}
        self.dmas = []
        self.res = {}
        self.dsem = {}
        self.nwait = 0
        self.nins = 0

    def R(self, key):
        r = self.res.get(key)
        if r is None:
            r = self.res[key] = Res()
        return r

    def _add(self, deps, o, d, kind):
        if (not d.dma) and (not o.dma) and d.eng == o.eng:
            if o.eng == "pe":
                return
        deps.append(d)

    @staticmethod
    def _ispsum(k):
        return isinstance(k, str) and len(k) == 2 and k[0] == "P" and k[1].isdigit()

    def _deps(self, o, r, w):
        pr = [k for k in r if self._ispsum(k)]
        if pr:
            r = [k for k in r if not self._ispsum(k)]
            w = list(w) + [k for k in pr if k not in w]
        deps = []
        key = o.eng if not o.dma else ("dma", id(o))
        for k in r:
            x = self.R(k)
            for d in x.ws.values():
                self._add(deps, o, d, "raw")
        for k in w:
            x = self.R(k)
            for d in x.ws.values():
                self._add(deps, o, d, "waw")
            for d in x.rs.values():
                self._add(deps, o, d, "war")
        for k in r:
            self.R(k).rs[key] = o
        for k in w:
            x = self.R(k)
            if x.rs:
                x.ws = {}
                x.rs = {}
            x.ws[key] = o
        return deps

    def _wait(self, eng, deps):
        sn = self.seen[eng]
        for d in deps:
            if d.alias is not None:
                d = d.alias
            assert d.sem is not None, "dependency on unsignalled op"
            k = id(d.sem)
            if sn.get(k, 0) < d.val:
                self.h[eng].wait_ge(d.sem, d.val)
                sn[k] = d.val
                self.nwait += 1

    def op(self, eng, fn, r=(), w=(), sig=True):
        o = Op(eng)
        deps = self._deps(o, r, w)
        self._wait(eng, deps)
        ins = fn()
        self.nins += 1
        if sig:
            self.cnt[eng] += 1
            o.sem = self.esem[eng]
            o.val = self.cnt[eng]
            ins.then_inc(o.sem, 1)
            for p in self.pend[eng]:
                p.alias = o
            self.pend[eng] = []
        else:
            self.pend[eng].append(o)
        self.last[eng] = o
        return o

    def dma(self, q, sem, pairs, r=(), w=(), extra=(), track=True, **kw):
        S = self.dsem.get(sem)
        if S is None:
            S = self.dsem[sem] = [self.es.enter_context(self.nc.semaphore("D_" + sem)), 0, None]
        o = Op(q, dma=True)
        deps = self._deps(o, r, w) + list(extra)
        if S[2] is not None:
            deps.append(S[2])
        self._wait(q, deps)
        S[1] += 16 * len(pairs)
        o.sem = S[0]
        o.val = S[1]
        for (out, in_) in pairs:
            self.h[q].dma_start(out=out, in_=in_, **kw).then_inc(S[0], 16)
            self.nins += 1
        S[2] = o
        if track:
            self.dmas.append(o)
        return o

    def barrier(self):
        deps = [self.last[e] for e in self.esem if self.last[e] is not None] + self.dmas
        for e in self.h:
            self._wait(e, deps)
        self.dmas = []
        self.res = {}

    def finish(self):
        self._wait("sp", list(self.dmas))


class _Stop(Exception):
    pass


def build(n_layers=4, dbg=False, n_seq=2, stop=None):
    nc = bass.Bass("TRN2", target_bir_lowering=False)

    def din(name, shape, dt=F32):
        return nc.dram_tensor(name, shape, dt, kind="ExternalInput").ap()

    def dint(name, shape, dt):
        return nc.dram_tensor(name, shape, dt, kind="Internal").ap()

    x_d = din("x", [2, S_, D])
    meta_d = din("meta", [NMETA, D])
    w1_d = din("mlp_w1", [4, D, DFF])
    w2_d = din("mlp_w2", [4, DFF, D])
    win_d = din("a_w_in", [2, D, 2120])
    wout_d = din("a_w_out", [2, D, D])
    kvw_d = din("kv_w", [D, 2064])
    bwq_d = din("b_w_q", [2, D, D])
    bwout_d = din("b_w_out", [2, D, D])
    cst_d = din("cst", [128, C_END])
    gv_d = din("gv", [128, G_END])
    out_d = nc.dram_tensor("out", [2, S_, D], F32, kind="ExternalOutput").ap()
    dbg_d = None
    if dbg:
        dbg_d = nc.dram_tensor("dbg", [8, 128, T_], F32, kind="ExternalOutput").ap()

    w1b = dint("w1b", [4, D, DFF], BF16)
    w2b = dint("w2b", [4, DFF, D], BF16)
    winb = dint("winb", [2, D, 2120], BF16)
    woutb = dint("woutb", [2, D, D], BF16)
    kvwb = dint("kvwb", [D, 2064], BF16)
    bwqb = dint("bwqb", [2, D, D], BF16)
    bwoutb = dint("bwoutb", [2, D, D], BF16)
    hT_all = dint("hT_d", [2, 8, 128, T_], F32)
    hT_d = hT_all[0]
    qT_d = dint("qT_d", [8, 128, T_], BF16)
    qiT_d = dint("qiT_d", [4, 128, T_], BF16)
    qaug_d = dint("qaug_d", [16, 70, T_], BF16)
    KT_d = dint("KT_d", [16, 70, T_], BF16)
    Va_d = dint("Va_d", [16, 128, NB, 128], BF16)

    es = ExitStack()
    with es:
        T = Trk(nc, es)

        uniq = [0]

        done = [False]

        def chk(k):
            if stop == k:
                T.barrier()
                done[0] = True
            return done[0]

        def sb(st, name, shape, dt):
            uniq[0] += 1
            return st.enter_context(nc.sbuf_tensor(f"{name}_u{uniq[0]}", shape, dt))

        XT = sb(es, "XT", [128, 8, T_], BF16)
        WR = [sb(es, f"WR{i}", [128, 4096], BF16) for i in range(4)]
        cstf = sb(es, "cstf", [128, C_END - C_ID], F32)
        cstb = sb(es, "cstb", [128, 6 * 128], BF16)
        gv = sb(es, "gv", [128, G_END], F32)
        gq8 = sb(es, "gq8", [128, 2], F32)
        nfb = sb(es, "nfb", [128, 1], F32)
        epsb = sb(es, "epsb", [128, 1], F32)
        thrneg = sb(es, "thrneg", [128, 1], F32)
        PS = [es.enter_context(nc.psum_tensor(f"PS{i}", [128, 512], F32)) for i in range(8)]

        identf = cstf[:, 0:128]
        pow2 = cstf[:, C_POW - C_ID:C_POW - C_ID + 32]
        identb = cstb[:, 0:128]
        bonesb = cstb[:, 128:256]
        pswb = cstb[:, 256:384]
        triqsb = cstb[:, 384:512]
        triTb = cstb[:, 512:640]
        onesb = cstb[:, 640:768]

        wr_i = [0]

        wconv_ops = {}

        def wload(pairs_fn, wkey):
            i = wr_i[0] % len(WR)
            wr_i[0] += 1
            T.dma("sp", f"wr{i}", pairs_fn(WR[i]), w=(("WR", i),), extra=(wconv_ops[wkey],))
            return i

        def mm(out, lhsT, rhs, start, stop, r, w, sig=None):
            if sig is None:
                sig = stop
            return T.op("pe", lambda: nc.tensor.matmul(out, lhsT=lhsT, rhs=rhs, start=start, stop=stop), r, w, sig)

        def act(out, in_, func, r, w, **kw):
            return T.op("act", lambda: nc.scalar.activation(out=out, in_=in_, func=func, **kw), r, w)

        def ts(eng, out, in0, s1, s2, op0, op1, r, w, **kw):
            h = nc.vector if eng == "dve" else nc.gpsimd
            if op1 is None:
                return T.op(eng, lambda: h.tensor_scalar(out=out, in0=in0, scalar1=s1, scalar2=None, op0=op0, **kw), r, w)
            return T.op(eng, lambda: h.tensor_scalar(out=out, in0=in0, scalar1=s1, scalar2=s2, op0=op0, op1=op1, **kw), r, w)

        def tt(eng, out, in0, in1, op, r, w):
            h = nc.vector if eng == "dve" else nc.gpsimd
            return T.op(eng, lambda: h.tensor_tensor(out=out, in0=in0, in1=in1, op=op), r, w)

        def stt(out, in0, scalar, in1, op0, op1, r, w):
            return T.op("dve", lambda: nc.vector.scalar_tensor_tensor(out=out, in0=in0, scalar=scalar, in1=in1,
                                                                      op0=op0, op1=op1), r, w)

        def recip(out, in_, r, w):
            return T.op("dve", lambda: nc.vector.reciprocal(out=out, in_=in_), r, w)

        def cp(eng, out, in_, r, w):
            if eng == "act":
                return T.op("act", lambda: nc.scalar.copy(out=out, in_=in_), r, w)
            h = nc.vector if eng == "dve" else nc.gpsimd
            return T.op(eng, lambda: h.tensor_copy(out=out, in_=in_), r, w)

        def k8(ap, k=8):
            return ap.rearrange("p (k n) -> p k n", k=k)

        T.dma("sp", "cst", [(cstf[:], cst_d[:, C_ID:C_END]), (gv[:], gv_d[:, :])], w=("cstf", "gv"))
        conv = [("win0", winb[0], win_d[0]), ("wout0", woutb[0], wout_d[0]), ("w1_0", w1b[0], w1_d[0]), ("w2_0", w2b[0], w2_d[0]),
                ("win1", winb[1], win_d[1]), ("wout1", woutb[1], wout_d[1]), ("w1_1", w1b[1], w1_d[1]), ("w2_1", w2b[1], w2_d[1]),
                ("kvw", kvwb, kvw_d), ("bwq0", bwqb[0], bwq_d[0]), ("bwout0", bwoutb[0], bwout_d[0]),
                ("w1_2", w1b[2], w1_d[2]), ("w2_2", w2b[2], w2_d[2]), ("bwq1", bwqb[1], bwq_d[1]), ("bwout1", bwoutb[1], bwout_d[1]),
                ("w1_3", w1b[3], w1_d[3]), ("w2_3", w2b[3], w2_d[3])]
        cp("dve", cstb[:, 0:640], cstf[:, 0:640], r=("cstf",), w=("cstb",))
        cp("dve", cstb[:, 640:768], cstf[:, C_ONES - C_ID:C_ONES - C_ID + 128], r=("cstf",), w=("cstb",))
        ts("dve", gq8[:], gv[:, G_BQ:G_BQ + 2], 0.125, None, ALU.mult, None, r=("gv",), w=("gq8",))
        ts("dve", nfb[:], gv[:, G_FB:G_FB + 1], -1.0, None, ALU.mult, None, r=("gv",), w=("nfb",))
        T.op("pool", lambda: nc.gpsimd.memset(epsb[:], EPS), w=("epsb",))
        T.op("pool", lambda: nc.gpsimd.memset(thrneg[:], -1e29), w=("thrneg",))
        T.barrier()
        for (wk, dst_, src_) in conv:
            wconv_ops[wk] = T.dma("pool", "wc_" + wk, [(dst_, src_)], track=(wk in ("win0", "wout0", "w1_0", "w2_0")))

        def norm_cols(src_fn, src_res, gcol, c0, n, tmp):
            sq, std, rstd = tmp
            pb = PS[7]
            for kc in range(8):
                act(sq[:, kc, 0:n], src_fn(kc), AF.Square, r=src_res, w=(("nsq", kc),))
            for kc in range(8):
                mm(pb[:, 0:n], onesb, sq[:, kc, 0:n], kc == 0, kc == 7, r=(("nsq", kc),), w=("P7",))
            act(std[:, 0:n], pb[:, 0:n], AF.Ln, r=("P7",), w=("nstd",), bias=epsb[:, 0:1], scale=1.0 / D)
            act(rstd[:, 0:n], std[:, 0:n], AF.Exp, r=("nstd",), w=("nrstd",), scale=-0.5)
            for kc in range(8):
                stt(XT[:, kc, c0:c0 + n], src_fn(kc), gv[:, gcol + kc:gcol + kc + 1], rstd[:, 0:n], ALU.mult, ALU.mult,
                    r=tuple(src_res) + ("nrstd",), w=(("XT", kc, c0),))

        def norm_from_dram(gcol):
            with ExitStack() as ph:
                hc = [sb(ph, f"n_hc{i}", [128, 8, 512], F32) for i in range(2)]
                sq = sb(ph, "n_sq", [128, 8, 512], BF16)
                std = sb(ph, "n_std", [128, 512], F32)
                rstd = sb(ph, "n_rstd", [128, 512], F32)
                for ci, (c0, n) in enumerate(CH):
                    b = hc[ci % 2]
                    T.dma("sp", f"nhc{ci % 2}", [(b[:, :, 0:n], hT_d[:, :, c0:c0 + n].rearrange("k p n -> p k n"))],
                          w=(("nhc", ci % 2),))
                    norm_cols(lambda kc, b=b, n=n: b[:, kc, 0:n], (("nhc", ci % 2),), gcol, c0, n, (sq, std, rstd))
                T.barrier()

        def input_phase(s):
            with ExitStack() as ph:
                xin = [sb(ph, f"i_x{i}", [128, D], F32) for i in range(2)]
                stg = [sb(ph, f"i_s{i}", [128, 8, 512], F32) for i in range(2)]
                for ci, (c0, n) in enumerate(CH):
                    st = stg[ci % 2]
                    for bb in range(n // 128):
                        b = c0 // 128 + bb
                        xb = xin[b % 2]
                        rk = ("ix", b % 2)
                        if b == 0:
                            T.dma("sp", f"ix{b % 2}", [(xb[0:16, :], meta_d[:, :]), (xb[16:128, :], x_d[s, 0:112, :])], w=(rk,))
                        elif b < 16:
                            T.dma("sp", f"ix{b % 2}", [(xb[:, :], x_d[s, 128 * b - 16:128 * b + 112, :])], w=(rk,))
                        else:
                            T.op("pool", lambda xb=xb: nc.gpsimd.memset(xb[:, :], 0.0), w=(rk,))
                            T.dma("sp", f"ix{b % 2}", [(xb[0:16, :], x_d[s, 2032:2048, :])], w=(rk,))
                        for half in range(2):
                            pidx = (2 * b + half) % 4
                            pb = PS[pidx]
                            pk = f"P{pidx}"
                            for j in range(4):
                                kc = half * 4 + j
                                T.op("pe", lambda pb=pb, j=j, kc=kc, xb=xb: nc.tensor.transpose(
                                    out=pb[:, j * 128:(j + 1) * 128], in_=xb[:, kc * 128:(kc + 1) * 128], identity=identf),
                                    r=(rk,), w=(pk,), sig=(j == 3))
                            eng = "act" if half == 0 else "dve"
                            cp(eng, st[:, half * 4:half * 4 + 4, bb * 128:(bb + 1) * 128],
                               k8(pb[:, :], 4), r=(pk,), w=(("ist", ci % 2),))
                    T.dma("sp", f"ist{ci % 2}", [(hT_d[:, :, c0:c0 + n].rearrange("k p n -> p k n"), st[:, :, 0:n])],
                          r=(("ist", ci % 2),))
                T.barrier()

        def feat_proj(wsrc, colbase, ntiles, Mt, epi, wkey):
            wcols = ntiles * Mt
            i = wload(lambda s_: [(k8(s_[:, 0:8 * wcols]), wsrc[:, :, colbase:colbase + wcols])], wkey)
            wt = k8(WR[i][:, 0:8 * wcols])
            pending = None
            for t_ in range(ntiles):
                for ci, (c0, n) in enumerate(CH):
                    pidx = (t_ * 5 + ci) % 4
                    pb = PS[pidx]
                    pk = f"P{pidx}"
                    for kc in range(8):
                        mm(pb[0:Mt, 0:n], wt[:, kc, t_ * Mt:(t_ + 1) * Mt], XT[:, kc, c0:c0 + n], kc == 0, kc == 7,
                           r=(("WR", i), ("XT", kc, c0)), w=(pk,))
                    if pending is not None:
                        epi(*pending)
                    pending = (t_, ci, c0, n, pb, pk)
            if pending is not None:
                epi(*pending)

        def alloc_epi(ph):
            d = {}
            d["sqb"] = sb(ph, "e_sqb", [128, 512], BF16)
            d["qb"] = sb(ph, "e_qb", [128, 512], BF16)
            d["std"] = sb(ph, "e_std", [128, 512], F32)
            d["rstd"] = sb(ph, "e_rstd", [128, 512], F32)
            d["t1"] = sb(ph, "e_t1", [128, 512], F32)
            d["t2"] = sb(ph, "e_t2", [128, 512], F32)
            d["o"] = [sb(ph, f"e_o{i}", [128, 512], BF16) for i in range(2)]
            d["oi"] = 0
            return d

        def head_rstd(E, pb, pk, n, P=128):
            act(E["sqb"][0:P, 0:n], pb[0:P, 0:n], AF.Square, r=(pk,), w=("e_sq",))
            mm(PS[6][0:P, 0:n], bonesb[0:P, 0:P], E["sqb"][0:P, 0:n], True, True, r=("e_sq",), w=("P6",))
            act(E["std"][0:P, 0:n], PS[6][0:P, 0:n], AF.Ln, r=("P6",), w=("e_std",), bias=epsb[0:P, 0:1], scale=1.0 / 64)
            act(E["rstd"][0:P, 0:n], E["std"][0:P, 0:n], AF.Exp, r=("e_std",), w=("e_rstd",), scale=-0.5)

        def rope_epi(E, pb, pk, n, c0, P, gcol, has_norm, outs, wres, cosT, sinT):
            cp("act", E["qb"][0:P, 0:n], pb[0:P, 0:n], r=(pk,), w=("e_qb",))
            mm(PS[5][0:P, 0:n], pswb[0:P, 0:P], E["qb"][0:P, 0:n], True, True, r=("e_qb",), w=("P5",))
            t1, t2 = E["t1"], E["t2"]
            if has_norm:
                head_rstd(E, pb, pk, n, P)
                stt(t1[0:P, 0:n], pb[0:P, 0:n], gv[0:P, gcol:gcol + 1], cosT[0:P, c0:c0 + n], ALU.mult, ALU.mult,
                    r=(pk, "cos"), w=("e_t1",))
                stt(t2[0:P, 0:n], PS[5][0:P, 0:n], gv[0:P, gcol + 1:gcol + 2], sinT[0:P, c0:c0 + n], ALU.mult, ALU.mult,
                    r=("P5", "cos"), w=("e_t2",))
                tt("pool", t1[0:P, 0:n], t1[0:P, 0:n], t2[0:P, 0:n], ALU.add, r=("e_t1", "e_t2"), w=("e_t1",))
                for (o_ap, sl) in outs:
                    tt("dve", o_ap, t1[sl, 0:n], E["rstd"][sl, 0:n], ALU.mult, r=("e_t1", "e_rstd"), w=wres)
            else:
                stt(t1[0:P, 0:n], pb[0:P, 0:n], 1.0, cosT[0:P, c0:c0 + n], ALU.mult, ALU.mult, r=(pk, "cos"), w=("e_t1",))
                stt(t2[0:P, 0:n], PS[5][0:P, 0:n], 1.0, sinT[0:P, c0:c0 + n], ALU.mult, ALU.mult, r=("P5", "cos"), w=("e_t2",))
                for (o_ap, sl) in outs:
                    tt("dve", o_ap, t1[sl, 0:n], t2[sl, 0:n], ALU.add, r=("e_t1", "e_t2"), w=wres)

        def headnorm_store_epi(E, gain_ap, dst_d, tbase):
            def epi(t_, ci, c0, n, pb, pk):
                head_rstd(E, pb, pk, n)
                oi = E["oi"] % 2
                E["oi"] += 1
                o = E["o"][oi]
                stt(o[:, 0:n], pb[:, 0:n], gain_ap, E["rstd"][:, 0:n], ALU.mult, ALU.mult, r=(pk, "e_rstd"), w=(("e_o", oi),))
                hd = 2 * (tbase + t_)
                T.dma("sp", f"eo{oi}", [(dst_d[hd, 0:64, c0:c0 + n], o[0:64, 0:n]),
                                          (dst_d[hd + 1, 0:64, c0:c0 + n], o[64:128, 0:n])], r=(("e_o", oi),))
            return epi

        def run_pipe(steps, L):
            st = {}
            n = len(steps)
            for k in range(n + L):
                if k < n:
                    st[k] = steps[k][0]()
                if k - L >= 0:
                    steps[k - L][1](st.pop(k - L))

        def attn_A(KTd, Vau, kiTd, absw, sgn):
            with ExitStack() as ph:
                qTc = [sb(ph, f"a_qTc{i}", [128, 8, 512], BF16) for i in range(2)]
                qiTc = [sb(ph, f"a_qiTc{i}", [128, 4, 512], BF16) for i in range(2)]
                score = [sb(ph, f"a_score{i}", [128, T_], F32) for i in range(2)]
                mb = [sb(ph, f"a_mb{i}", [128, T_], BF16) for i in range(2)]
                mbT = [sb(ph, f"a_mbT{i}", [128, NB, 128], BF16) for i in range(2)]
                Rb = [sb(ph, f"a_Rb{i}", [128, 512], BF16) for i in range(3)]
                PT = [sb(ph, f"a_PT{i}", [128, 512], BF16) for i in range(4)]
                Dm = [sb(ph, f"a_Dm{i}", [128, 8, 128], BF16) for i in range(2)]
                rden = [sb(ph, f"a_rden{i}", [128, 512], F32) for i in range(2)]
                sm = [sb(ph, f"a_sm{i}", [128, 8], F32) for i in range(2)]
                dks = [sb(ph, f"a_dks{i}", [128, 32], F32) for i in range(2)]
                TRb = PS[7][:, :].bitcast(BF16)
                SB = [0, 1, 5]
                loaded = set()

                def load_chunk(c):
                    if c in loaded:
                        return
                    loaded.add(c)
                    cc0, cn = CH[c]
                    T.dma("sp", f"qc{c % 2}",
                          [(qTc[c % 2][:, :, 0:cn], qT_d[:, :, cc0:cc0 + cn].rearrange("k p n -> p k n")),
                           (qiTc[c % 2][:, :, 0:cn], qiT_d[:, :, cc0:cc0 + cn].rearrange("k p n -> p k n"))], w=(("qc", c % 2),))

                def idx_gen(i):
                    p = i % 2
                    c = i // 4
                    qoff = (i % 4) * 128
                    qk = ("qc", c % 2)
                    load_chunk(c)
                    qiT = qiTc[c % 2]
                    for h in range(8):
                        ts("pool", Dm[p][:, h, :], identb, sgn[:, i, h:h + 1], None, ALU.mult, None, w=(("Dm", p, h),), r=())
                    nk = (i + 1) * 128
                    nkc = (nk + 511) // 512
                    for kk in range(nkc):
                        k0 = kk * 512
                        ncol = min(512, nk - k0)
                        last = kk == nkc - 1
                        SC = PS[4]
                        Lp = PS[3]

                        def sc_mm(h):
                            mm(SC[:, 0:ncol], Dm[p][:, h, :], Rb[h % 3][:, 0:ncol], h == 0, (h == 7 and not last),
                               r=(("Dm", p, h), ("Rb", h % 3)), w=("P4",))
                        for h in range(8):
                            if h > 0:
                                sc_mm(h - 1)
                            mm(Lp[:, 0:ncol], qiT[:, h // 2, qoff:qoff + 128], kiTd[:, h % 2, k0:k0 + ncol],
                               True, True, r=(qk,), w=("P3",))
                            act(Rb[h % 3][:, 0:ncol], Lp[:, 0:ncol], AF.Relu, r=("P3",), w=(("Rb", h % 3),),
                                scale=absw[:, i, h:h + 1])
                            yield
                        sc_mm(7)
                        if last:
                            dc = nk - 128 - k0
                            mm(SC[:, dc:dc + 128], identb, triqsb, False, True, r=(), w=("P4",))
                        cp("dve", score[p][:, k0:k0 + ncol], SC[:, 0:ncol], r=("P4",), w=(("score", p),))
                        yield

                def bis_gen(i):
                    p = i % 2
                    nk = (i + 1) * 128
                    hi, lo, dd, mid, cnt, tq = (sm[p][:, k:k + 1] for k in range(6))
                    if i >= 2:
                        T.op("dve", lambda: nc.vector.tensor_reduce(out=hi, in_=score[p][:, 0:nk], axis=AX.X, op=ALU.max),
                             r=(("score", p),), w=(("hi", p),))
                        T.op("dve", lambda: nc.vector.tensor_reduce(out=lo, in_=score[p][:, 0:256], axis=AX.X, op=ALU.min),
                             r=(("score", p),), w=(("lo", p),))
                        tt("dve", dd, hi, lo, ALU.subtract, r=(("hi", p), ("lo", p)), w=(("dd", p),))
                        ts("dve", dks[p][:, 0:NBIS], pow2[:, 0:NBIS], dd, None, ALU.mult, None, r=(("dd", p),), w=(("dks", p),))
                        yield
                        for k in range(NBIS):
                            tt("dve", mid, lo, dks[p][:, k:k + 1], ALU.add, r=(("lo", p), ("dks", p)), w=(("mid", p),))
                            ts("dve", mb[p][:, 0:nk], score[p][:, 0:nk], mid, None, ALU.is_ge, ALU.add,
                               r=(("score", p), ("mid", p)), w=(("mb", p), ("cnt", p)), accum_out=cnt)
                            stt(tq, cnt, 255.5, dks[p][:, k:k + 1], ALU.is_ge, ALU.mult, r=(("cnt", p), ("dks", p)), w=(("tq", p),))
                            tt("dve", lo, lo, tq, ALU.add, r=(("lo", p), ("tq", p)), w=(("lo", p),))
                            yield
                        thr = lo
                    else:
                        thr = thrneg[:, 0:1]
                    ts("dve", mb[p][:, 0:nk], score[p][:, 0:nk], thr, NEG, ALU.is_lt, ALU.mult, r=(("score", p), ("lo", p)),
                       w=(("mb", p),))
                    yield
                    for j0 in range(0, i + 1, 4):
                        nj = min(4, i + 1 - j0)
                        for jj in range(nj):
                            T.op("pe", lambda jj=jj, j0=j0: nc.tensor.transpose(
                                out=TRb[:, jj * 128:(jj + 1) * 128], in_=mb[p][:, (j0 + jj) * 128:(j0 + jj + 1) * 128],
                                identity=identb), r=(("mb", p),), w=("P7",), sig=(jj == nj - 1))
                        cp("dve", mbT[p][:, j0:j0 + nj, :], k8(TRb[:, 0:nj * 128], nj), r=("P7",), w=(("mbT", p),))
                        yield

                def n_idx(i):
                    return 9 * (((i + 1) * 128 + 511) // 512)

                def n_bis(i):
                    return (NBIS + 1 if i >= 2 else 0) + 1 + (i + 4) // 4

                def drain(gen):
                    if gen is not None:
                        for _ in gen:
                            pass

                drain(idx_gen(0))
                drain(bis_gen(0))
                drain(idx_gen(1))
                scn = [0]
                for i in range(NB):
                    p = i % 2
                    c = i // 4
                    cc0, cn = CH[c]
                    qoff = (i % 4) * 128
                    qk = ("qc", c % 2)
                    qT = qTc[c % 2]
                    gA = idx_gen(i + 2) if i + 2 < NB else None
                    gB = bis_gen(i + 1) if i + 1 < NB else None
                    nA = n_idx(i + 2) if gA is not None else 0
                    nB = n_bis(i + 1) if gB is not None else 0
                    nst = 4 * (i + 1)
                    mcount = [0]

                    def weave():
                        m = mcount[0]
                        mcount[0] += 1
                        for (gen, ntot) in ((gB, nB), (gA, nA)):
                            if gen is None:
                                continue
                            k = (ntot * (m + 1) + nst - 1) // nst - (ntot * m + nst - 1) // nst
                            for _ in range(k):
                                try:
                                    next(gen)
                                except StopIteration:
                                    break
                    steps = []
                    for g in range(4):
                        O = PS[6 if g % 2 == 0 else 2]
                        ok = "P6" if g % 2 == 0 else "P2"
                        for j in range(i + 1):
                            def s_fn(g=g, j=j):
                                k_ = scn[0]
                                scn[0] += 1
                                sbk = SB[k_ % 3]
                                Sp = PS[sbk]
                                sk = f"P{sbk}"
                                pt = PT[k_ % 4]
                                ptk = ("PT", k_ % 4)
                                js = slice(j * 128, (j + 1) * 128)
                                weave()
                                mm(k8(Sp[:, 0:512], 4), identb, mbT[p][:, j:j + 1, :].broadcast_to([128, 4, 128]), True, False,
                                   r=(("mbT", p),), w=(sk,))
                                mm(k8(Sp[:, 0:256], 2), KTd[:, 0, g, js], qT[:, 2 * g:2 * g + 2, qoff:qoff + 128], False, False,
                                   r=(qk,), w=(sk,))
                                mm(k8(Sp[:, 256:512], 2), KTd[:, 1, g, js], qT[:, 2 * g:2 * g + 2, qoff:qoff + 128], False, True,
                                   r=(qk,), w=(sk,))
                                act(pt[:, :], Sp[:, :], AF.Exp, r=(sk,), w=(ptk,), scale=0.125)
                                return pt, ptk

                            def v_fn(st, g=g, j=j, O=O, ok=ok):
                                pt, ptk = st
                                mm(O[:, :], Vau[:, j, g, :], pt[:, :], j == 0, j == i, r=(ptk,), w=(ok,))
                                if j == i:
                                    rd = rden[g % 2]
                                    rk = ("rden", g % 2)
                                    act(rd[0:64, :], O[64:128, :], AF.Ln, r=(ok,), w=(rk,))
                                    act(rd[0:64, :], rd[0:64, :], AF.Exp, r=(rk,), w=(rk,), scale=-1.0)
                                    qs = slice(cc0 + qoff, cc0 + qoff + 128)
                                    tt("dve", XT[0:64, 2 * g:2 * g + 2, qs], k8(O[0:64, 0:256], 2), k8(rd[0:64, 0:256], 2), ALU.mult,
                                       r=(ok, rk), w=(("XT", 2 * g, cc0), ("XT", 2 * g + 1, cc0)))
                                    tt("dve", XT[64:128, 2 * g:2 * g + 2, qs], k8(O[0:64, 256:512], 2), k8(rd[0:64, 256:512], 2),
                                       ALU.mult, r=(ok, rk), w=(("XT", 2 * g, cc0), ("XT", 2 * g + 1, cc0)))
                            steps.append((s_fn, v_fn))
                    run_pipe(steps, 2)
                    drain(gB)
                    drain(gA)
                T.barrier()

        def layer_A(l):
            with ExitStack() as pa:
                KTd = sb(pa, "a_KTd", [128, 2, 4, T_], BF16)
                Vau = sb(pa, "a_Vau", [128, NB, 4, 128], BF16)
                kiTd = sb(pa, "a_kiTd", [128, 2, T_], BF16)
                T.op("pool", lambda: nc.gpsimd.memset(KTd[64:128, 0, :, :], 0.0), w=("KTz0",))
                T.op("pool", lambda: nc.gpsimd.memset(KTd[0:64, 1, :, :], 0.0), w=("KTz1",))
                T.op("pool", lambda: nc.gpsimd.memset(kiTd[64:128, 0, :], 0.0), w=("kiz0",))
                T.op("pool", lambda: nc.gpsimd.memset(kiTd[0:64, 1, :], 0.0), w=("kiz1",))
                wi = sb(pa, "a_wi", [128, NB, 8], F32)
                absw = sb(pa, "a_absw", [128, NB, 8], F32)
                sgn = sb(pa, "a_sgn", [128, NB, 8], F32)
                T.op("pool", lambda: nc.gpsimd.memset(Vau[:, :, :, 64:128], 1.0), w=("Vau1",))
                norm_from_dram(G_ATT + 8 * l)
                if chk(2):
                    return
                with ExitStack() as ph:
                    cs = sb(ph, "a_cs", [128, 2 * T_], F32)
                    cosT = cs[:, 0:T_]
                    sinT = cs[:, T_:2 * T_]
                    T.dma("sp", "cs", [(cs[:], cst_d[:, 0:2 * T_])], w=("cos",))
                    E = alloc_epi(ph)
                    wv = winb[l].rearrange("(k p) n -> p k n", p=128)

                    def q_epi(tbase, dst, has_norm, gcol):
                        def epi(t_, ci, c0, n, pb, pk):
                            oi = E["oi"] % 2
                            E["oi"] += 1
                            o = E["o"][oi]
                            rope_epi(E, pb, pk, n, c0, 128, gcol, has_norm, [(o[:, 0:n], slice(0, 128))], (("e_o", oi),), cosT, sinT)
                            T.dma("sp", f"eo{oi}", [(dst[tbase + t_, :, c0:c0 + n], o[:, 0:n])], r=(("e_o", oi),))
                        return epi

                    def k_epi(t_, ci, c0, n, pb, pk):
                        g0, g1 = 2 * t_, 2 * t_ + 1
                        outs = [(KTd[0:64, 0, g0, c0:c0 + n], slice(0, 64)), (KTd[64:128, 1, g0, c0:c0 + n], slice(0, 64)),
                                (KTd[64:128, 1, g1, c0:c0 + n], slice(64, 128)), (KTd[0:64, 0, g1, c0:c0 + n], slice(64, 128))]
                        rope_epi(E, pb, pk, n, c0, 128, G_AK + 2 * l, True, outs, ("KTd",), cosT, sinT)

                    def ki_epi(t_, ci, c0, n, pb, pk):
                        outs = [(kiTd[0:64, 0, c0:c0 + n], slice(0, 64)), (kiTd[64:128, 1, c0:c0 + n], slice(0, 64))]
                        rope_epi(E, pb, pk, n, c0, 64, 0, False, outs, ("kiTd",), cosT, sinT)

                    feat_proj(wv, 0, 4, 128, q_epi(0, qT_d, True, G_AQ + 2 * l), f"win{l}")
                    if chk(21):
                        return
                    feat_proj(wv, 512, 4, 128, q_epi(4, qT_d, True, G_AQ + 2 * l), f"win{l}")
                    feat_proj(wv, 1024, 2, 128, k_epi, f"win{l}")
                    if chk(22):
                        return
                    feat_proj(wv, 1536, 4, 128, q_epi(0, qiT_d, False, 0), f"win{l}")
                    if chk(23):
                        return
                    feat_proj(wv, 2048, 1, 64, ki_epi, f"win{l}")
                    if chk(24):
                        return
                    i = wload(lambda s_: [(k8(s_[:, 0:8 * 264])[:, :, 0:256], wv[:, :, 1280:1536]),
                                          (k8(s_[:, 0:8 * 264])[:, :, 256:264], wv[:, :, 2112:2120])], f"win{l}")
                    wt = k8(WR[i][:, 0:8 * 264])
                    for b in range(NB):
                        pb = PS[b % 4]
                        pk = f"P{b % 4}"
                        c0 = CH[b // 4][0]
                        for kc in range(8):
                            mm(pb[:, 0:264], XT[:, kc, b * 128:(b + 1) * 128], wt[:, kc, :], kc == 0, kc == 7,
                               r=(("WR", i), ("XT", kc, c0)), w=(pk,))
                        cp("act" if b % 2 == 0 else "dve", Vau[:, b, :, 0:64], k8(pb[:, 0:256], 4), r=(pk,), w=("Vau",))
                        cp("dve", wi[:, b, :], pb[:, 256:264], r=(pk,), w=("wi",))
                    act(absw[:], wi[:], AF.Abs, r=("wi",), w=("absw",), scale=IDXSCALE)
                    ts("dve", sgn[:], wi[:], 0.0, 2.0, ALU.is_ge, ALU.mult, r=("wi",), w=("sgn",))
                    ts("dve", sgn[:], sgn[:], -1.0, None, ALU.add, None, r=("sgn",), w=("sgn",))
                    T.barrier()
                if chk(3):
                    return
                attn_A(KTd, Vau, kiTd, absw, sgn)

        def kv_phase2():
            norm_from_dram(G_KVN)
            kvv = kvwb.rearrange("(k p) n -> p k n", p=128)
            with ExitStack() as ph:
                fl = sb(ph, "k_fl", [16, T_], F32)
                with ExitStack() as ph2:
                    E = alloc_epi(ph2)
                    Vst = [sb(ph2, f"k_Vst{i}", [128, 8, 4, 128], BF16) for i in range(2)]
                    for i_ in range(2):
                        T.op("pool", lambda i_=i_: nc.gpsimd.memset(Vst[i_][:, :, :, 64:128], 1.0), w=(("Vst", i_),))
                    feat_proj(kvv, 0, 4, 128, headnorm_store_epi(E, gv[:, G_KVK:G_KVK + 1], KT_d, 0), "kvw")
                    feat_proj(kvv, 512, 4, 128, headnorm_store_epi(E, gv[:, G_KVK:G_KVK + 1], KT_d, 4), "kvw")
                    vi = 0
                    for half in range(2):
                        i = wload(lambda s_: [(k8(s_[:, :]), kvv[:, :, 1024 + 512 * half:1536 + 512 * half])], "kvw")
                        wt = k8(WR[i][:, :])
                        for b in range(NB):
                            pb = PS[b % 4]
                            pk = f"P{b % 4}"
                            ci = b // 4
                            bb = b % 4
                            c0, n = CH[ci]
                            if bb == 0:
                                vi += 1
                            st = Vst[vi % 2]
                            for kc in range(8):
                                mm(pb[:, :], XT[:, kc, b * 128:(b + 1) * 128], wt[:, kc, :], kc == 0, kc == 7,
                                   r=(("WR", i), ("XT", kc, c0)), w=(pk,))
                            cp("act" if b % 2 == 0 else "dve", st[:, :, bb, 0:64], k8(pb[:, :], 8), r=(pk,), w=(("Vst", vi % 2),))
                            if bb == n // 128 - 1:
                                nb_ = n // 128
                                T.dma("sp", f"vst{vi % 2}",
                                      [(Va_d[8 * half + hh, :, 4 * ci:4 * ci + nb_, :], st[:, hh, 0:nb_, :]) for hh in range(8)],
                                      r=(("Vst", vi % 2),))

                    def f_epi(t_, ci, c0, n, pb, pk):
                        cp("dve", fl[0:16, c0:c0 + n], pb[0:16, 0:n], r=(pk,), w=("fl",))
                    feat_proj(kvv, 2048, 1, 16, f_epi, "kvw")
                    T.barrier()
                on = sb(ph, "k_on", [16, T_], F32)
                cc = sb(ph, "k_c", [16, T_], F32)
                r1 = sb(ph, "k_r1", [16, T_], F32)
                qa = sb(ph, "k_qa", [16, 6, T_], BF16)
                ka = sb(ph, "k_ka", [16, 6, T_], BF16)
                T.op("pool", lambda: nc.gpsimd.memset(on[:, :], 1.0), w=("on",))
                T.op("pool", lambda: nc.gpsimd.memset(qa[:, 3:6, :], 1.0), w=("qa1",))
                T.op("pool", lambda: nc.gpsimd.memset(ka[:, 0:3, :], 1.0), w=("ka1",))
                act(r1[:, :], fl[:, :], AF.Exp, r=("fl",), w=("r1",), scale=-1.0, bias=nfb[0:16, 0:1])
                act(fl[:, :], r1[:, :], AF.Ln, r=("r1",), w=("fl",), bias=1.0)
                T.op("dve", lambda: nc.vector.tensor_tensor_scan(out=cc[:, :], data0=on[:, :], data1=fl[:, :], initial=0.0,
                                                                 op0=ALU.mult, op1=ALU.subtract), r=("on", "fl"), w=("cc",))
                cp("dve", qa[:, 0, :], cc[:, :], r=("cc",), w=("qa0",))
                tt("dve", r1[:, :], cc[:, :], qa[:, 0, :], ALU.subtract, r=("cc", "qa0"), w=("r1",))
                cp("dve", qa[:, 1, :], r1[:, :], r=("r1",), w=("qa1b",))
                tt("dve", fl[:, :], r1[:, :], qa[:, 1, :], ALU.subtract, r=("r1", "qa1b"), w=("fl",))
                cp("dve", qa[:, 2, :], fl[:, :], r=("fl",), w=("qa2",))
                ts("dve", ka[:, 3:6, :], qa[:, 0:3, :], -1.0, None, ALU.mult, None, r=("qa0", "qa1b", "qa2"), w=("ka2",))
                T.dma("sp", "caug", [(qaug_d[:, 64:70, :], qa[:, :, :]), (KT_d[:, 64:70, :], ka[:, :, :])],
                      r=("qa0", "qa1b", "qa2", "qa1", "ka1", "ka2"))
                T.barrier()

        def attn_B():
            with ExitStack() as ph:
                slots = [(sb(ph, f"b_qh{i}", [70, T_], BF16), sb(ph, f"b_kh{i}", [70, T_], BF16),
                          sb(ph, f"b_vh{i}", [128, NB, 128], BF16)) for i in range(3)]
                PT = [sb(ph, f"b_PT{i}", [128, 512], BF16) for i in range(6)]
                SBK = [0, 1, 2, 3, 4]
                scn = [0]
                rden = [sb(ph, f"b_rden{i}", [128, 512], F32) for i in range(2)]
                oc = 0
                for h in range(16):
                    sl = h % 3
                    qh, kh, vh = slots[sl]
                    hk = ("bh", sl)
                    T.dma("sp", f"bh{sl}", [(qh[:, :], qaug_d[h]), (kh[:, :], KT_d[h]), (vh[:, :, :], Va_d[h])], w=(hk,))
                    steps = []
                    for ci, (c0, n) in enumerate(CH):
                        O = PS[6 if oc % 2 == 0 else 5]
                        ok = "P6" if oc % 2 == 0 else "P5"
                        rd = rden[oc % 2]
                        rk = ("rden", oc % 2)
                        oc += 1
                        b0 = c0 // 128
                        jlast = b0 + n // 128 - 1
                        for j in range(jlast + 1):
                            rr = max(j - b0, 0)
                            col0 = c0 + rr * 128
                            ncols = c0 + n - col0
                            diag = j >= b0

                            def s_fn(j=j, col0=col0, ncols=ncols, diag=diag):
                                k_ = scn[0]
                                scn[0] += 1
                                Sp = PS[SBK[k_ % 5]]
                                sk = f"P{SBK[k_ % 5]}"
                                pt = PT[k_ % 6]
                                ptk = ("PT", k_ % 6)
                                mm(Sp[:, 0:ncols], kh[0:70, j * 128:(j + 1) * 128], qh[0:70, col0:col0 + ncols], True, not diag,
                                   r=(hk,), w=(sk,))
                                if diag:
                                    mm(Sp[:, 0:128], identb, triTb, False, True, r=(), w=(sk,))
                                act(pt[:, 0:ncols], Sp[:, 0:ncols], AF.Exp, r=(sk,), w=(ptk,))
                                return pt, ptk

                            def v_fn(st, j=j, col0=col0, ncols=ncols, c0=c0, n=n, O=O, ok=ok, rd=rd, rk=rk, jlast=jlast, h=h):
                                pt, ptk = st
                                mm(O[:, col0 - c0:col0 - c0 + ncols], vh[:, j, :], pt[:, 0:ncols], j == 0, j == jlast,
                                   r=(ptk, hk), w=(ok,))
                                if j == jlast:
                                    recip(rd[0:64, 0:n], O[64:128, 0:n], r=(ok,), w=(rk,))
                                    pbase = (h % 2) * 64
                                    tt("dve", XT[pbase:pbase + 64, h // 2, c0:c0 + n], O[0:64, 0:n], rd[0:64, 0:n], ALU.mult,
                                       r=(ok, rk), w=(("XT", h // 2, c0),))
                            steps.append((s_fn, v_fn))
                    run_pipe(steps, 3)
                T.barrier()

        def layer_B(j):
            norm_from_dram(G_ATT + 8 * (2 + j))
            with ExitStack() as ph:
                E = alloc_epi(ph)
                wv = bwqb[j].rearrange("(k p) n -> p k n", p=128)
                feat_proj(wv, 0, 4, 128, headnorm_store_epi(E, gq8[:, j:j + 1], qaug_d, 0), f"bwq{j}")
                feat_proj(wv, 512, 4, 128, headnorm_store_epi(E, gq8[:, j:j + 1], qaug_d, 4), f"bwq{j}")
                T.barrier()
            attn_B()

        def outproj_mlp(l, wo_d, s, last, wokey):
            with ExitStack() as ph:
                hTf = sb(ph, "m_hTf", [128, 8, T_], F32)
                for ci, (c0, n) in enumerate(CH):
                    T.dma("sp", "hload", [(hTf[:, :, c0:c0 + n], hT_d[:, :, c0:c0 + n].rearrange("k p n -> p k n"))],
                          w=tuple(("hTf", m, c0) for m in range(8)))
                wov = wo_d.rearrange("(k p) n -> p k n", p=128)

                def o_epi(tbase):
                    def epi(t_, ci, c0, n, pb, pk):
                        m = tbase + t_
                        tt("dve", hTf[:, m, c0:c0 + n], hTf[:, m, c0:c0 + n], pb[:, 0:n], ALU.add, r=(pk, ("hTf", m, c0)),
                           w=(("hTf", m, c0),))
                    return epi
                feat_proj(wov, 0, 4, 128, o_epi(0), wokey)
                feat_proj(wov, 512, 4, 128, o_epi(4), wokey)
                with ExitStack() as ph2:
                    sq = sb(ph2, "n_sq", [128, 8, 512], BF16)
                    std = sb(ph2, "n_std", [128, 512], F32)
                    rstd = sb(ph2, "n_rstd", [128, 512], F32)
                    for ci, (c0, n) in enumerate(CH):
                        norm_cols(lambda kc, c0=c0, n=n: hTf[:, kc, c0:c0 + n], tuple(("hTf", m, c0) for m in range(8)),
                                  G_MLP + 8 * l, c0, n, (sq, std, rstd))
                    T.barrier()
                with ExitStack() as ph2:
                    aT = [sb(ph2, f"m_aT{i}", [128, 4, T_], BF16) for i in range(2)]
                    rt = [sb(ph2, f"m_rt{i}", [128, 512], F32) for i in range(2)]
                    w1v = w1b[l].rearrange("(k p) n -> p k n", p=128)
                    w2v = w2b[l].rearrange("(f p) n -> p f n", p=128)
                    ri = 0
                    pc = 0
                    yc = 0
                    for fg in range(8):
                        i1 = wload(lambda s_: [(k8(s_[:, :]), w1v[:, :, fg * 512:(fg + 1) * 512])], f"w1_{l}")
                        w1t = k8(WR[i1][:, :])
                        a = aT[fg % 2]
                        for f in range(4):
                            for ci, (c0, n) in enumerate(CH):
                                pidx = pc % 4
                                pc += 1
                                pb = PS[pidx]
                                pk = f"P{pidx}"
                                for kc in range(8):
                                    mm(pb[:, 0:n], w1t[:, kc, f * 128:(f + 1) * 128], XT[:, kc, c0:c0 + n], kc == 0, kc == 7,
                                       r=(("WR", i1), ("XT", kc, c0)), w=(pk,))
                                r_ = rt[ri % 2]
                                rk = ("rt", ri % 2)
                                ri += 1
                                act(r_[:, 0:n], pb[:, 0:n], AF.Relu, r=(pk,), w=(rk,))
                                tt("pool", a[:, f, c0:c0 + n], r_[:, 0:n], r_[:, 0:n], ALU.mult, r=(rk,), w=(("aT", fg % 2, f, c0),))
                        i2 = wload(lambda s_: [(k8(s_[:, :], 4), w2v[:, fg * 4:(fg + 1) * 4, :])], f"w2_{l}")
                        w2t = k8(WR[i2][:, :], 4)
                        for m in range(8):
                            for ci, (c0, n) in enumerate(CH):
                                pidx = 4 + yc % 4
                                yc += 1
                                pb = PS[pidx]
                                pk = f"P{pidx}"
                                for f in range(4):
                                    mm(pb[:, 0:n], w2t[:, f, m * 128:(m + 1) * 128], a[:, f, c0:c0 + n], f == 0, f == 3,
                                       r=(("WR", i2), ("aT", fg % 2, f, c0)), w=(pk,))
                                tt("dve", hTf[:, m, c0:c0 + n], hTf[:, m, c0:c0 + n], pb[:, 0:n], ALU.add,
                                   r=(pk, ("hTf", m, c0)), w=(("hTf", m, c0),))
                    T.barrier()
                if not last:
                    for ci, (c0, n) in enumerate(CH):
                        T.dma("sp", "hstore", [(hT_d[:, :, c0:c0 + n].rearrange("k p n -> p k n"), hTf[:, :, c0:c0 + n])])
                    if dbg_d is not None and s == 0:
                        T.dma("sp", "dbgst", [(dbg_d[:, :, :].rearrange("k p n -> p k n"), hTf[:, :, :])])
                else:
                    if dbg_d is not None and s == 0:
                        T.dma("sp", "dbgst", [(dbg_d[:, :, :].rearrange("k p n -> p k n"), hTf[:, :, :])])
                    with ExitStack() as ph2:
                        og = [sb(ph2, f"o_st{i}", [128, D], F32) for i in range(2)]
                        for b in range(NB):
                            o = og[b % 2]
                            okk = ("ost", b % 2)
                            for half in range(2):
                                pidx = (2 * b + half) % 4
                                pb = PS[pidx]
                                pk = f"P{pidx}"
                                for jq in range(4):
                                    kc = half * 4 + jq
                                    T.op("pe", lambda pb=pb, jq=jq, kc=kc, b=b: nc.tensor.transpose(
                                        out=pb[:, jq * 128:(jq + 1) * 128], in_=hTf[:, kc, b * 128:(b + 1) * 128], identity=identf),
                                        r=(), w=(pk,), sig=(jq == 3))
                                cp("act" if half == 0 else "dve", o[:, half * 512:(half + 1) * 512], pb[:, :], r=(pk,), w=(okk,))
                            if b == 0:
                                T.dma("sp", f"ost{b % 2}", [(out_d[s, 0:112, :], o[16:128, :])], r=(okk,))
                            elif b < 16:
                                T.dma("sp", f"ost{b % 2}", [(out_d[s, 128 * b - 16:128 * b + 112, :], o[:, :])], r=(okk,))
                            else:
                                T.dma("sp", f"ost{b % 2}", [(out_d[s, 2032:2048, :], o[0:16, :])], r=(okk,))
                        T.barrier()
                T.barrier()

        chk(0)
        for s in range(n_seq):
            if done[0]:
                break
            hT_d = hT_all[s]
            input_phase(s)
        chk(1)
        for s in range(n_seq):
            if done[0]:
                break
            hT_d = hT_all[s]
            for l in range(n_layers):
                last = l == n_layers - 1
                if l < 2:
                    layer_A(l)
                    if done[0] or chk(4):
                        break
                    outproj_mlp(l, woutb[l], s, last, f"wout{l}")
                else:
                    if l == 2:
                        kv_phase2()
                    layer_B(l - 2)
                    outproj_mlp(l, bwoutb[l - 2], s, last, f"bwout{l - 2}")
        T.finish()
        stats = (T.nins, T.nwait, dict(T.cnt))
    return nc, stats


def host_consts():
    inv = (1.0 / (np.float32(10000.0) ** (np.arange(0, 64, 2, dtype=np.float32) / np.float32(64)))).astype(np.float32)
    ang = (np.arange(T_, dtype=np.float32)[:, None] * inv[None, :]).astype(np.float32)
    cos = np.cos(ang).astype(np.float32)
    sin = np.sin(ang).astype(np.float32)
    cst = np.zeros((128, C_END), np.float32)
    p = np.arange(128)
    d = p % 64
    cst[:, C_COS:C_COS + T_] = cos[:, d % 32].T
    sg = np.where(d < 32, -1.0, 1.0).astype(np.float32)
    cst[:, C_SIN:C_SIN + T_] = sin[:, d % 32].T * sg[:, None]
    cst[:, C_ID:C_ID + 128] = np.eye(128, dtype=np.float32)
    cst[:, C_BONES:C_BONES + 128] = (p[:, None] // 64 == p[None, :] // 64).astype(np.float32)
    cst[:, C_PSW:C_PSW + 128] = (p[:, None] == (p[None, :] ^ 32)).astype(np.float32)
    cst[:, C_TRIQS:C_TRIQS + 128] = np.where(p[None, :] > p[:, None], -1e30, 0.0).astype(np.float32)
    cst[:, C_TRIT:C_TRIT + 128] = np.where(p[:, None] > p[None, :], NEG, 0.0).astype(np.float32)
    cst[:, C_POW:C_POW + 32] = (0.5 ** np.arange(1, 33, dtype=np.float64)).astype(np.float32)[None, :]
    cst[:, C_ONES:C_ONES + 128] = 1.0
    return cst


def host_gv(attn_norm, mlp_norm, kv_norm, a_q_gain, a_k_gain, kv_k_gain, b_q_gain, kv_f_bias):
    gv = np.zeros((128, G_END), np.float32)
    p = np.arange(128)
    d = p % 64
    for l in range(4):
        gv[:, G_ATT + 8 * l:G_ATT + 8 * l + 8] = np.asarray(attn_norm[l]).reshape(8, 128).T
        gv[:, G_MLP + 8 * l:G_MLP + 8 * l + 8] = np.asarray(mlp_norm[l]).reshape(8, 128).T
    gv[:, G_KVN:G_KVN + 8] = np.asarray(kv_norm).reshape(8, 128).T
    for l in range(2):
        gv[:, G_AQ + 2 * l] = np.asarray(a_q_gain[l])[d]
        gv[:, G_AQ + 2 * l + 1] = np.asarray(a_q_gain[l])[d ^ 32]
        gv[:, G_AK + 2 * l] = np.asarray(a_k_gain[l])[d]
        gv[:, G_AK + 2 * l + 1] = np.asarray(a_k_gain[l])[d ^ 32]
        gv[:, G_BQ + l] = np.asarray(b_q_gain[l])[d]
    gv[:, G_KVK] = np.asarray(kv_k_gain)[d]
    gv[0:16, G_FB] = np.asarray(kv_f_bias)
    return gv


_CACHE = {}


def kernel(x, meta_tokens, attn_norm, mlp_norm, mlp_w1, mlp_w2, a_w_in, a_q_gain, a_k_gain, a_w_out,
           kv_norm, kv_w, kv_f_bias, kv_k_gain, b_w_q, b_q_gain, b_w_out, _n_layers=4, _dbg=False, _n_seq=2, _trace=False, _stop=None, _cores=8):
    f = lambda a: np.ascontiguousarray(np.asarray(a, dtype=np.float32))
    x = f(x)
    key = (_n_layers, _dbg, _n_seq, _stop)
    if key not in _CACHE:
        _CACHE[key] = build(_n_layers, _dbg, _n_seq, _stop)
    nc, stats = _CACHE[key]
    shared = {
        "meta": f(meta_tokens), "mlp_w1": f(mlp_w1), "mlp_w2": f(mlp_w2), "a_w_in": f(a_w_in), "a_w_out": f(a_w_out),
        "kv_w": f(kv_w), "b_w_q": f(b_w_q), "b_w_out": f(b_w_out), "cst": host_consts(),
        "gv": host_gv(f(attn_norm), f(mlp_norm), f(kv_norm), f(a_q_gain), f(a_k_gain), f(kv_k_gain), f(b_q_gain), f(kv_f_bias)),
    }
    in_maps = []
    for c in range(_cores):
        m = dict(shared)
        m["x"] = np.ascontiguousarray(x[2 * c:2 * c + 2])
        in_maps.append(m)
    if _trace:
        res = run_bass_kernel_spmd(nc, in_maps, core_ids=list(range(_cores)), trace=True)
        print('exec_time_ns', res.exec_time_ns)
    else:
        res = run_bass_kernel_spmd(nc, in_maps, core_ids=list(range(_cores)))
    out = np.concatenate([np.asarray(r["out"]) for r in res.results], axis=0).astype(np.float32)
    if _dbg:
        return out, [np.asarray(r["dbg"]) for r in res.results]
    return out
```

```python
import numpy as np
from contextlib import ExitStack
import concourse.bass as bass
import concourse.mybir as mybir
from concourse.bass_utils import run_bass_kernel_spmd

F32 = mybir.dt.float32
BF16 = mybir.dt.bfloat16
ALU = mybir.AluOpType
AF = mybir.ActivationFunctionType
AX = mybir.AxisListType

D = 1024
S_ = 2048
NMETA = 16
T_ = 2176
NB = 17
DFF = 4096
CH = [(0, 512), (512, 512), (1024, 512), (1536, 512), (2048, 128)]
EPS = 1e-6
NBIS = 16
NEG = -30000.0
IDXSCALE = float(8 ** -0.5 * 64 ** -0.5)
C_COS, C_SIN, C_ID, C_BONES, C_PSW, C_TRIQS, C_TRIT, C_POW, C_ONES = (
    0, T_, 2 * T_, 2 * T_ + 128, 2 * T_ + 256, 2 * T_ + 384, 2 * T_ + 512, 2 * T_ + 640, 2 * T_ + 672)
C_END = C_ONES + 128
G_ATT, G_MLP, G_KVN, G_AQ, G_AK, G_KVK, G_BQ, G_FB, G_END = 0, 32, 64, 72, 76, 80, 81, 83, 84


class Op:
    __slots__ = ("eng", "dma", "sem", "val", "alias")

    def __init__(self, eng, dma=False):
        self.eng = eng
        self.dma = dma
        self.sem = None
        self.val = 0
        self.alias = None


class Res:
    __slots__ = ("ws", "rs")

    def __init__(self):
        self.ws = {}
        self.rs = {}


class Trk:
    def __init__(self, nc, es):
        self.nc = nc
        self.es = es
        self.h = {"pe": nc.tensor, "act": nc.scalar, "dve": nc.vector, "pool": nc.gpsimd, "sp": nc.sync}
        self.esem = {e: es.enter_context(nc.semaphore("S_" + e)) for e in ("pe", "act", "dve", "pool")}
        self.cnt = {e: 0 for e in self.esem}
        self.seen = {e: {} for e in self.h}
        self.pend = {e: [] for e in self.esem}
        self.last = {e: None for e in self.esem}
        self.dmas = []
        self.res = {}
        self.dsem = {}
        self.nwait = 0
        self.nins = 0

    def R(self, key):
        r = self.res.get(key)
        if r is None:
            r = self.res[key] = Res()
        return r

    def _add(self, deps, o, d, kind):
        if (not d.dma) and (not o.dma) and d.eng == o.eng:
            if o.eng == "pe":
                return
        deps.append(d)

    @staticmethod
    def _ispsum(k):
        return isinstance(k, str) and len(k) == 2 and k[0] == "P" and k[1].isdigit()

    def _deps(self, o, r, w):
        pr = [k for k in r if self._ispsum(k)]
        if pr:
            r = [k for k in r if not self._ispsum(k)]
            w = list(w) + [k for k in pr if k not in w]
        deps = []
        key = o.eng if not o.dma else ("dma", id(o))
        for k in r:
            x = self.R(k)
            for d in x.ws.values():
                self._add(deps, o, d, "raw")
        for k in w:
            x = self.R(k)
            for d in x.ws.values():
                self._add(deps, o, d, "waw")
            for d in x.rs.values():
                self._add(deps, o, d, "war")
        for k in r:
            self.R(k).rs[key] = o
        for k in w:
            x = self.R(k)
            if x.rs:
                x.ws = {}
                x.rs = {}
            x.ws[key] = o
        return deps

    def _wait(self, eng, deps):
        sn = self.seen[eng]
        for d in deps:
            if d.alias is not None:
                d = d.alias
            assert d.sem is not None, "dependency on unsignalled op"
            k = id(d.sem)
            if sn.get(k, 0) < d.val:
                self.h[eng].wait_ge(d.sem, d.val)
                sn[k] = d.val
                self.nwait += 1

    def op(self, eng, fn, r=(), w=(), sig=True):
        o = Op(eng)
        deps = self._deps(o, r, w)
        self._wait(eng, deps)
        ins = fn()
        self.nins += 1
        if sig:
            self.cnt[eng] += 1
            o.sem = self.esem[eng]
            o.val = self.cnt[eng]
            ins.then_inc(o.sem, 1)
            for p in self.pend[eng]:
                p.alias = o
            self.pend[eng] = []
        else:
            self.pend[eng].append(o)
        self.last[eng] = o
        return o

    def dma(self, q, sem, pairs, r=(), w=(), extra=(), track=True, **kw):
        S = self.dsem.get(sem)
        if S is None:
            S = self.dsem[sem] = [self.es.enter_context(self.nc.semaphore("D_" + sem)), 0, None]
        o = Op(q, dma=True)
        deps = self._deps(o, r, w) + list(extra)
        if S[2] is not None:
            deps.append(S[2])
        self._wait(q, deps)
        S[1] += 16 * len(pairs)
        o.sem = S[0]
        o.val = S[1]
        for (out, in_) in pairs:
            self.h[q].dma_start(out=out, in_=in_, **kw).then_inc(S[0], 16)
            self.nins += 1
        S[2] = o
        if track:
            self.dmas.append(o)
        return o

    def barrier(self):
        deps = [self.last[e] for e in self.esem if self.last[e] is not None] + self.dmas
        for e in self.h:
            self._wait(e, deps)
        self.dmas = []
        self.res = {}

    def finish(self):
        self._wait("sp", list(self.dmas))


class _Stop(Exception):
    pass


def build(n_layers=4, dbg=False, n_seq=2, stop=None):
    nc = bass.Bass("TRN2", target_bir_lowering=False)

    def din(name, shape, dt=F32):
        return nc.dram_tensor(name, shape, dt, kind="ExternalInput").ap()

    def dint(name, shape, dt):
        return nc.dram_tensor(name, shape, dt, kind="Internal").ap()

    x_d = din("x", [2, S_, D])
    meta_d = din("meta", [NMETA, D])
    w1_d = din("mlp_w1", [4, D, DFF])
    w2_d = din("mlp_w2", [4, DFF, D])
    win_d = din("a_w_in", [2, D, 2120])
    wout_d = din("a_w_out", [2, D, D])
    kvw_d = din("kv_w", [D, 2064])
    bwq_d = din("b_w_q", [2, D, D])
    bwout_d = din("b_w_out", [2, D, D])
    cst_d = din("cst", [128, C_END])
    gv_d = din("gv", [128, G_END])
    out_d = nc.dram_tensor("out", [2, S_, D], F32, kind="ExternalOutput").ap()
    dbg_d = None
    if dbg:
        dbg_d = nc.dram_tensor("dbg", [8, 128, T_], F32, kind="ExternalOutput").ap()

    w1b = dint("w1b", [4, D, DFF], BF16)
    w2b = dint("w2b", [4, DFF, D], BF16)
    winb = dint("winb", [2, D, 2120], BF16)
    woutb = dint("woutb", [2, D, D], BF16)
    kvwb = dint("kvwb", [D, 2064], BF16)
    bwqb = dint("bwqb", [2, D, D], BF16)
    bwoutb = dint("bwoutb", [2, D, D], BF16)
    hT_all = dint("hT_d", [2, 8, 128, T_], F32)
    hT_d = hT_all[0]
    qT_d = dint("qT_d", [8, 128, T_], BF16)
    qiT_d = dint("qiT_d", [4, 128, T_], BF16)
    qaug_d = dint("qaug_d", [16, 70, T_], BF16)
    KT_d = dint("KT_d", [16, 70, T_], BF16)
    Va_d = dint("Va_d", [16, 128, NB, 128], BF16)

    es = ExitStack()
    with es:
        T = Trk(nc, es)

        uniq = [0]

        done = [False]

        def chk(k):
            if stop == k:
                T.barrier()
                done[0] = True
            return done[0]

        def sb(st, name, shape, dt):
            uniq[0] += 1
            return st.enter_context(nc.sbuf_tensor(f"{name}_u{uniq[0]}", shape, dt))

        XT = sb(es, "XT", [128, 8, T_], BF16)
        WR = [sb(es, f"WR{i}", [128, 4096], BF16) for i in range(4)]
        cstf = sb(es, "cstf", [128, C_END - C_ID], F32)
        cstb = sb(es, "cstb", [128, 6 * 128], BF16)
        gv = sb(es, "gv", [128, G_END], F32)
        gq8 = sb(es, "gq8", [128, 2], F32)
        nfb = sb(es, "nfb", [128, 1], F32)
        epsb = sb(es, "epsb", [128, 1], F32)
        thrneg = sb(es, "thrneg", [128, 1], F32)
        PS = [es.enter_context(nc.psum_tensor(f"PS{i}", [128, 512], F32)) for i in range(8)]

        identf = cstf[:, 0:128]
        pow2 = cstf[:, C_POW - C_ID:C_POW - C_ID + 32]
        identb = cstb[:, 0:128]
        bonesb = cstb[:, 128:256]
        pswb = cstb[:, 256:384]
        triqsb = cstb[:, 384:512]
        triTb = cstb[:, 512:640]
        onesb = cstb[:, 640:768]

        wr_i = [0]

        wconv_ops = {}

        def wload(pairs_fn, wkey):
            i = wr_i[0] % len(WR)
            wr_i[0] += 1
            T.dma("sp", f"wr{i}", pairs_fn(WR[i]), w=(("WR", i),), extra=(wconv_ops[wkey],))
            return i

        def mm(out, lhsT, rhs, start, stop, r, w, sig=None):
            if sig is None:
                sig = stop
            return T.op("pe", lambda: nc.tensor.matmul(out, lhsT=lhsT, rhs=rhs, start=start, stop=stop), r, w, sig)

        def act(out, in_, func, r, w, **kw):
            return T.op("act", lambda: nc.scalar.activation(out=out, in_=in_, func=func, **kw), r, w)

        def ts(eng, out, in0, s1, s2, op0, op1, r, w, **kw):
            h = nc.vector if eng == "dve" else nc.gpsimd
            if op1 is None:
                return T.op(eng, lambda: h.tensor_scalar(out=out, in0=in0, scalar1=s1, scalar2=None, op0=op0, **kw), r, w)
            return T.op(eng, lambda: h.tensor_scalar(out=out, in0=in0, scalar1=s1, scalar2=s2, op0=op0, op1=op1, **kw), r, w)

        def tt(eng, out, in0, in1, op, r, w):
            h = nc.vector if eng == "dve" else nc.gpsimd
            return T.op(eng, lambda: h.tensor_tensor(out=out, in0=in0, in1=in1, op=op), r, w)

        def stt(out, in0, scalar, in1, op0, op1, r, w):
            return T.op("dve", lambda: nc.vector.scalar_tensor_tensor(out=out, in0=in0, scalar=scalar, in1=in1,
                                                                      op0=op0, op1=op1), r, w)

        def recip(out, in_, r, w):
            return T.op("dve", lambda: nc.vector.reciprocal(out=out, in_=in_), r, w)

        def cp(eng, out, in_, r, w):
            if eng == "act":
                return T.op("act", lambda: nc.scalar.copy(out=out, in_=in_), r, w)
            h = nc.vector if eng == "dve" else nc.gpsimd
            return T.op(eng, lambda: h.tensor_copy(out=out, in_=in_), r, w)

        def k8(ap, k=8):
            return ap.rearrange("p (k n) -> p k n", k=k)

        T.dma("sp", "cst", [(cstf[:], cst_d[:, C_ID:C_END]), (gv[:], gv_d[:, :])], w=("cstf", "gv"))
        conv = [("win0", winb[0], win_d[0]), ("wout0", woutb[0], wout_d[0]), ("w1_0", w1b[0], w1_d[0]), ("w2_0", w2b[0], w2_d[0]),
                ("win1", winb[1], win_d[1]), ("wout1", woutb[1], wout_d[1]), ("w1_1", w1b[1], w1_d[1]), ("w2_1", w2b[1], w2_d[1]),
                ("kvw", kvwb, kvw_d), ("bwq0", bwqb[0], bwq_d[0]), ("bwout0", bwoutb[0], bwout_d[0]),
                ("w1_2", w1b[2], w1_d[2]), ("w2_2", w2b[2], w2_d[2]), ("bwq1", bwqb[1], bwq_d[1]), ("bwout1", bwoutb[1], bwout_d[1]),
                ("w1_3", w1b[3], w1_d[3]), ("w2_3", w2b[3], w2_d[3])]
        cp("dve", cstb[:, 0:640], cstf[:, 0:640], r=("cstf",), w=("cstb",))
        cp("dve", cstb[:, 640:768], cstf[:, C_ONES - C_ID:C_ONES - C_ID + 128], r=("cstf",), w=("cstb",))
        ts("dve", gq8[:], gv[:, G_BQ:G_BQ + 2], 0.125, None, ALU.mult, None, r=("gv",), w=("gq8",))
        ts("dve", nfb[:], gv[:, G_FB:G_FB + 1], -1.0, None, ALU.mult, None, r=("gv",), w=("nfb",))
        T.op("pool", lambda: nc.gpsimd.memset(epsb[:], EPS), w=("epsb",))
        T.op("pool", lambda: nc.gpsimd.memset(thrneg[:], -1e29), w=("thrneg",))
        T.barrier()
        for (wk, dst_, src_) in conv:
            wconv_ops[wk] = T.dma("pool", "wc_" + wk, [(dst_, src_)], track=(wk in ("win0", "wout0")))

        def norm_cols(src_fn, src_res, gcol, c0, n, tmp):
            sq, std, rstd = tmp
            pb = PS[7]
            for kc in range(8):
                act(sq[:, kc, 0:n], src_fn(kc), AF.Square, r=src_res, w=(("nsq", kc),))
            for kc in range(8):
                mm(pb[:, 0:n], onesb, sq[:, kc, 0:n], kc == 0, kc == 7, r=(("nsq", kc),), w=("P7",))
            act(std[:, 0:n], pb[:, 0:n], AF.Ln, r=("P7",), w=("nstd",), bias=epsb[:, 0:1], scale=1.0 / D)
            act(rstd[:, 0:n], std[:, 0:n], AF.Exp, r=("nstd",), w=("nrstd",), scale=-0.5)
            for kc in range(8):
                stt(XT[:, kc, c0:c0 + n], src_fn(kc), gv[:, gcol + kc:gcol + kc + 1], rstd[:, 0:n], ALU.mult, ALU.mult,
                    r=tuple(src_res) + ("nrstd",), w=(("XT", kc, c0),))

        def norm_from_dram(gcol):
            with ExitStack() as ph:
                hc = [sb(ph, f"n_hc{i}", [128, 8, 512], F32) for i in range(2)]
                sq = sb(ph, "n_sq", [128, 8, 512], BF16)
                std = sb(ph, "n_std", [128, 512], F32)
                rstd = sb(ph, "n_rstd", [128, 512], F32)
                for ci, (c0, n) in enumerate(CH):
                    b = hc[ci % 2]
                    T.dma("sp", f"nhc{ci % 2}", [(b[:, :, 0:n], hT_d[:, :, c0:c0 + n].rearrange("k p n -> p k n"))],
                          w=(("nhc", ci % 2),))
                    norm_cols(lambda kc, b=b, n=n: b[:, kc, 0:n], (("nhc", ci % 2),), gcol, c0, n, (sq, std, rstd))
                T.barrier()

        def input_phase(s):
            with ExitStack() as ph:
                xin = [sb(ph, f"i_x{i}", [128, D], F32) for i in range(2)]
                stg = [sb(ph, f"i_s{i}", [128, 8, 512], F32) for i in range(2)]
                for ci, (c0, n) in enumerate(CH):
                    st = stg[ci % 2]
                    for bb in range(n // 128):
                        b = c0 // 128 + bb
                        xb = xin[b % 2]
                        rk = ("ix", b % 2)
                        if b == 0:
                            T.dma("sp", f"ix{b % 2}", [(xb[0:16, :], meta_d[:, :]), (xb[16:128, :], x_d[s, 0:112, :])], w=(rk,))
                        elif b < 16:
                            T.dma("sp", f"ix{b % 2}", [(xb[:, :], x_d[s, 128 * b - 16:128 * b + 112, :])], w=(rk,))
                        else:
                            T.op("pool", lambda xb=xb: nc.gpsimd.memset(xb[:, :], 0.0), w=(rk,))
                            T.dma("sp", f"ix{b % 2}", [(xb[0:16, :], x_d[s, 2032:2048, :])], w=(rk,))
                        for half in range(2):
                            pidx = (2 * b + half) % 4
                            pb = PS[pidx]
                            pk = f"P{pidx}"
                            for j in range(4):
                                kc = half * 4 + j
                                T.op("pe", lambda pb=pb, j=j, kc=kc, xb=xb: nc.tensor.transpose(
                                    out=pb[:, j * 128:(j + 1) * 128], in_=xb[:, kc * 128:(kc + 1) * 128], identity=identf),
                                    r=(rk,), w=(pk,), sig=(j == 3))
                            eng = "act" if half == 0 else "dve"
                            cp(eng, st[:, half * 4:half * 4 + 4, bb * 128:(bb + 1) * 128],
                               k8(pb[:, :], 4), r=(pk,), w=(("ist", ci % 2),))
                    T.dma("sp", f"ist{ci % 2}", [(hT_d[:, :, c0:c0 + n].rearrange("k p n -> p k n"), st[:, :, 0:n])],
                          r=(("ist", ci % 2),))
                T.barrier()

        def feat_proj(wsrc, colbase, ntiles, Mt, epi, wkey):
            wcols = ntiles * Mt
            i = wload(lambda s_: [(k8(s_[:, 0:8 * wcols]), wsrc[:, :, colbase:colbase + wcols])], wkey)
            wt = k8(WR[i][:, 0:8 * wcols])
            pending = None
            for t_ in range(ntiles):
                for ci, (c0, n) in enumerate(CH):
                    pidx = (t_ * 5 + ci) % 4
                    pb = PS[pidx]
                    pk = f"P{pidx}"
                    for kc in range(8):
                        mm(pb[0:Mt, 0:n], wt[:, kc, t_ * Mt:(t_ + 1) * Mt], XT[:, kc, c0:c0 + n], kc == 0, kc == 7,
                           r=(("WR", i), ("XT", kc, c0)), w=(pk,))
                    if pending is not None:
                        epi(*pending)
                    pending = (t_, ci, c0, n, pb, pk)
            if pending is not None:
                epi(*pending)

        def alloc_epi(ph):
            d = {}
            d["sqb"] = sb(ph, "e_sqb", [128, 512], BF16)
            d["qb"] = sb(ph, "e_qb", [128, 512], BF16)
            d["std"] = sb(ph, "e_std", [128, 512], F32)
            d["rstd"] = sb(ph, "e_rstd", [128, 512], F32)
            d["t1"] = sb(ph, "e_t1", [128, 512], F32)
            d["t2"] = sb(ph, "e_t2", [128, 512], F32)
            d["o"] = [sb(ph, f"e_o{i}", [128, 512], BF16) for i in range(2)]
            d["oi"] = 0
            return d

        def head_rstd(E, pb, pk, n, P=128):
            act(E["sqb"][0:P, 0:n], pb[0:P, 0:n], AF.Square, r=(pk,), w=("e_sq",))
            mm(PS[6][0:P, 0:n], bonesb[0:P, 0:P], E["sqb"][0:P, 0:n], True, True, r=("e_sq",), w=("P6",))
            act(E["std"][0:P, 0:n], PS[6][0:P, 0:n], AF.Ln, r=("P6",), w=("e_std",), bias=epsb[0:P, 0:1], scale=1.0 / 64)
            act(E["rstd"][0:P, 0:n], E["std"][0:P, 0:n], AF.Exp, r=("e_std",), w=("e_rstd",), scale=-0.5)

        def rope_epi(E, pb, pk, n, c0, P, gcol, has_norm, outs, wres, cosT, sinT):
            cp("act", E["qb"][0:P, 0:n], pb[0:P, 0:n], r=(pk,), w=("e_qb",))
            mm(PS[5][0:P, 0:n], pswb[0:P, 0:P], E["qb"][0:P, 0:n], True, True, r=("e_qb",), w=("P5",))
            t1, t2 = E["t1"], E["t2"]
            if has_norm:
                head_rstd(E, pb, pk, n, P)
                stt(t1[0:P, 0:n], pb[0:P, 0:n], gv[0:P, gcol:gcol + 1], cosT[0:P, c0:c0 + n], ALU.mult, ALU.mult,
                    r=(pk, "cos"), w=("e_t1",))
                stt(t2[0:P, 0:n], PS[5][0:P, 0:n], gv[0:P, gcol + 1:gcol + 2], sinT[0:P, c0:c0 + n], ALU.mult, ALU.mult,
                    r=("P5", "cos"), w=("e_t2",))
                tt("pool", t1[0:P, 0:n], t1[0:P, 0:n], t2[0:P, 0:n], ALU.add, r=("e_t1", "e_t2"), w=("e_t1",))
                for (o_ap, sl) in outs:
                    tt("dve", o_ap, t1[sl, 0:n], E["rstd"][sl, 0:n], ALU.mult, r=("e_t1", "e_rstd"), w=wres)
            else:
                stt(t1[0:P, 0:n], pb[0:P, 0:n], 1.0, cosT[0:P, c0:c0 + n], ALU.mult, ALU.mult, r=(pk, "cos"), w=("e_t1",))
                stt(t2[0:P, 0:n], PS[5][0:P, 0:n], 1.0, sinT[0:P, c0:c0 + n], ALU.mult, ALU.mult, r=("P5", "cos"), w=("e_t2",))
                for (o_ap, sl) in outs:
                    tt("dve", o_ap, t1[sl, 0:n], t2[sl, 0:n], ALU.add, r=("e_t1", "e_t2"), w=wres)

        def headnorm_store_epi(E, gain_ap, dst_d, tbase):
            def epi(t_, ci, c0, n, pb, pk):
                head_rstd(E, pb, pk, n)
                oi = E["oi"] % 2
                E["oi"] += 1
                o = E["o"][oi]
                stt(o[:, 0:n], pb[:, 0:n], gain_ap, E["rstd"][:, 0:n], ALU.mult, ALU.mult, r=(pk, "e_rstd"), w=(("e_o", oi),))
                hd = 2 * (tbase + t_)
                T.dma("sp", f"eo{oi}", [(dst_d[hd, 0:64, c0:c0 + n], o[0:64, 0:n]),
                                          (dst_d[hd + 1, 0:64, c0:c0 + n], o[64:128, 0:n])], r=(("e_o", oi),))
            return epi

        def run_pipe(steps, L):
            st = {}
            n = len(steps)
            for k in range(n + L):
                if k < n:
                    st[k] = steps[k][0]()
                if k - L >= 0:
                    steps[k - L][1](st.pop(k - L))

        def attn_A(KTd, Vau, kiTd, absw, sgn):
            with ExitStack() as ph:
                qTc = [sb(ph, f"a_qTc{i}", [128, 8, 512], BF16) for i in range(2)]
                qiTc = [sb(ph, f"a_qiTc{i}", [128, 4, 512], BF16) for i in range(2)]
                score = [sb(ph, f"a_score{i}", [128, T_], F32) for i in range(2)]
                mb = [sb(ph, f"a_mb{i}", [128, T_], BF16) for i in range(2)]
                mbT = [sb(ph, f"a_mbT{i}", [128, NB, 128], BF16) for i in range(2)]
                Rb = [sb(ph, f"a_Rb{i}", [128, 512], BF16) for i in range(3)]
                PT = [sb(ph, f"a_PT{i}", [128, 512], BF16) for i in range(4)]
                Dm = [sb(ph, f"a_Dm{i}", [128, 8, 128], BF16) for i in range(2)]
                rden = [sb(ph, f"a_rden{i}", [128, 512], F32) for i in range(2)]
                sm = [sb(ph, f"a_sm{i}", [128, 8], F32) for i in range(2)]
                dks = [sb(ph, f"a_dks{i}", [128, 32], F32) for i in range(2)]
                TRb = PS[7][:, :].bitcast(BF16)
                SB = [0, 1, 5]
                loaded = set()

                def load_chunk(c):
                    if c in loaded:
                        return
                    loaded.add(c)
                    cc0, cn = CH[c]
                    T.dma("sp", f"qc{c % 2}",
                          [(qTc[c % 2][:, :, 0:cn], qT_d[:, :, cc0:cc0 + cn].rearrange("k p n -> p k n")),
                           (qiTc[c % 2][:, :, 0:cn], qiT_d[:, :, cc0:cc0 + cn].rearrange("k p n -> p k n"))], w=(("qc", c % 2),))

                def idx_gen(i):
                    p = i % 2
                    c = i // 4
                    qoff = (i % 4) * 128
                    qk = ("qc", c % 2)
                    load_chunk(c)
                    qiT = qiTc[c % 2]
                    for h in range(8):
                        ts("pool", Dm[p][:, h, :], identb, sgn[:, i, h:h + 1], None, ALU.mult, None, w=(("Dm", p, h),), r=())
                    nk = (i + 1) * 128
                    nkc = (nk + 511) // 512
                    for kk in range(nkc):
                        k0 = kk * 512
                        ncol = min(512, nk - k0)
                        last = kk == nkc - 1
                        SC = PS[4]
                        Lp = PS[3]

                        def sc_mm(h):
                            mm(SC[:, 0:ncol], Dm[p][:, h, :], Rb[h % 3][:, 0:ncol], h == 0, (h == 7 and not last),
                               r=(("Dm", p, h), ("Rb", h % 3)), w=("P4",))
                        for h in range(8):
                            if h > 0:
                                sc_mm(h - 1)
                            mm(Lp[:, 0:ncol], qiT[:, h // 2, qoff:qoff + 128], kiTd[:, h % 2, k0:k0 + ncol],
                               True, True, r=(qk,), w=("P3",))
                            act(Rb[h % 3][:, 0:ncol], Lp[:, 0:ncol], AF.Relu, r=("P3",), w=(("Rb", h % 3),),
                                scale=absw[:, i, h:h + 1])
                            yield
                        sc_mm(7)
                        if last:
                            dc = nk - 128 - k0
                            mm(SC[:, dc:dc + 128], identb, triqsb, False, True, r=(), w=("P4",))
                        cp("dve", score[p][:, k0:k0 + ncol], SC[:, 0:ncol], r=("P4",), w=(("score", p),))
                        yield

                def bis_gen(i):
                    p = i % 2
                    nk = (i + 1) * 128
                    hi, lo, dd, mid, cnt, tq = (sm[p][:, k:k + 1] for k in range(6))
                    if i >= 2:
                        T.op("dve", lambda: nc.vector.tensor_reduce(out=hi, in_=score[p][:, 0:nk], axis=AX.X, op=ALU.max),
                             r=(("score", p),), w=(("hi", p),))
                        T.op("dve", lambda: nc.vector.tensor_reduce(out=lo, in_=score[p][:, 0:256], axis=AX.X, op=ALU.min),
                             r=(("score", p),), w=(("lo", p),))
                        tt("dve", dd, hi, lo, ALU.subtract, r=(("hi", p), ("lo", p)), w=(("dd", p),))
                        ts("dve", dks[p][:, 0:NBIS], pow2[:, 0:NBIS], dd, None, ALU.mult, None, r=(("dd", p),), w=(("dks", p),))
                        yield
                        for k in range(NBIS):
                            tt("dve", mid, lo, dks[p][:, k:k + 1], ALU.add, r=(("lo", p), ("dks", p)), w=(("mid", p),))
                            ts("dve", mb[p][:, 0:nk], score[p][:, 0:nk], mid, None, ALU.is_ge, ALU.add,
                               r=(("score", p), ("mid", p)), w=(("mb", p), ("cnt", p)), accum_out=cnt)
                            stt(tq, cnt, 255.5, dks[p][:, k:k + 1], ALU.is_ge, ALU.mult, r=(("cnt", p), ("dks", p)), w=(("tq", p),))
                            tt("dve", lo, lo, tq, ALU.add, r=(("lo", p), ("tq", p)), w=(("lo", p),))
                            yield
                        thr = lo
                    else:
                        thr = thrneg[:, 0:1]
                    ts("dve", mb[p][:, 0:nk], score[p][:, 0:nk], thr, NEG, ALU.is_lt, ALU.mult, r=(("score", p), ("lo", p)),
                       w=(("mb", p),))
                    yield
                    for j0 in range(0, i + 1, 4):
                        nj = min(4, i + 1 - j0)
                        for jj in range(nj):
                            T.op("pe", lambda jj=jj, j0=j0: nc.tensor.transpose(
                                out=TRb[:, jj * 128:(jj + 1) * 128], in_=mb[p][:, (j0 + jj) * 128:(j0 + jj + 1) * 128],
                                identity=identb), r=(("mb", p),), w=("P7",), sig=(jj == nj - 1))
                        cp("dve", mbT[p][:, j0:j0 + nj, :], k8(TRb[:, 0:nj * 128], nj), r=("P7",), w=(("mbT", p),))
                        yield

                def n_idx(i):
                    return 9 * (((i + 1) * 128 + 511) // 512)

                def n_bis(i):
                    return (NBIS + 1 if i >= 2 else 0) + 1 + (i + 4) // 4

                def drain(gen):
                    if gen is not None:
                        for _ in gen:
                            pass

                drain(idx_gen(0))
                drain(bis_gen(0))
                drain(idx_gen(1))
                scn = [0]
                for i in range(NB):
                    p = i % 2
                    c = i // 4
                    cc0, cn = CH[c]
                    qoff = (i % 4) * 128
                    qk = ("qc", c % 2)
                    qT = qTc[c % 2]
                    gA = idx_gen(i + 2) if i + 2 < NB else None
                    gB = bis_gen(i + 1) if i + 1 < NB else None
                    nA = n_idx(i + 2) if gA is not None else 0
                    nB = n_bis(i + 1) if gB is not None else 0
                    nst = 4 * (i + 1)
                    mcount = [0]

                    def weave():
                        m = mcount[0]
                        mcount[0] += 1
                        for (gen, ntot) in ((gB, nB), (gA, nA)):
                            if gen is None:
                                continue
                            k = (ntot * (m + 1) + nst - 1) // nst - (ntot * m + nst - 1) // nst
                            for _ in range(k):
                                try:
                                    next(gen)
                                except StopIteration:
                                    break
                    steps = []
                    for g in range(4):
                        O = PS[6 if g % 2 == 0 else 2]
                        ok = "P6" if g % 2 == 0 else "P2"
                        for j in range(i + 1):
                            def s_fn(g=g, j=j):
                                k_ = scn[0]
                                scn[0] += 1
                                sbk = SB[k_ % 3]
                                Sp = PS[sbk]
                                sk = f"P{sbk}"
                                pt = PT[k_ % 4]
                                ptk = ("PT", k_ % 4)
                                js = slice(j * 128, (j + 1) * 128)
                                weave()
                                mm(k8(Sp[:, 0:512], 4), identb, mbT[p][:, j:j + 1, :].broadcast_to([128, 4, 128]), True, False,
                                   r=(("mbT", p),), w=(sk,))
                                mm(k8(Sp[:, 0:256], 2), KTd[:, 0, g, js], qT[:, 2 * g:2 * g + 2, qoff:qoff + 128], False, False,
                                   r=(qk,), w=(sk,))
                                mm(k8(Sp[:, 256:512], 2), KTd[:, 1, g, js], qT[:, 2 * g:2 * g + 2, qoff:qoff + 128], False, True,
                                   r=(qk,), w=(sk,))
                                act(pt[:, :], Sp[:, :], AF.Exp, r=(sk,), w=(ptk,), scale=0.125)
                                return pt, ptk

                            def v_fn(st, g=g, j=j, O=O, ok=ok):
                                pt, ptk = st
                                mm(O[:, :], Vau[:, j, g, :], pt[:, :], j == 0, j == i, r=(ptk,), w=(ok,))
                                if j == i:
                                    rd = rden[g % 2]
                                    rk = ("rden", g % 2)
                                    act(rd[0:64, :], O[64:128, :], AF.Ln, r=(ok,), w=(rk,))
                                    act(rd[0:64, :], rd[0:64, :], AF.Exp, r=(rk,), w=(rk,), scale=-1.0)
                                    qs = slice(cc0 + qoff, cc0 + qoff + 128)
                                    tt("dve", XT[0:64, 2 * g:2 * g + 2, qs], k8(O[0:64, 0:256], 2), k8(rd[0:64, 0:256], 2), ALU.mult,
                                       r=(ok, rk), w=(("XT", 2 * g, cc0), ("XT", 2 * g + 1, cc0)))
                                    tt("dve", XT[64:128, 2 * g:2 * g + 2, qs], k8(O[0:64, 256:512], 2), k8(rd[0:64, 256:512], 2),
                                       ALU.mult, r=(ok, rk), w=(("XT", 2 * g, cc0), ("XT", 2 * g + 1, cc0)))
                            steps.append((s_fn, v_fn))
                    run_pipe(steps, 2)
                    drain(gB)
                    drain(gA)
                T.barrier()

        def layer_A(l):
            with ExitStack() as pa:
                KTd = sb(pa, "a_KTd", [128, 2, 4, T_], BF16)
                Vau = sb(pa, "a_Vau", [128, NB, 4, 128], BF16)
                kiTd = sb(pa, "a_kiTd", [128, 2, T_], BF16)
                T.op("pool", lambda: nc.gpsimd.memset(KTd[64:128, 0, :, :], 0.0), w=("KTz0",))
                T.op("pool", lambda: nc.gpsimd.memset(KTd[0:64, 1, :, :], 0.0), w=("KTz1",))
                T.op("pool", lambda: nc.gpsimd.memset(kiTd[64:128, 0, :], 0.0), w=("kiz0",))
                T.op("pool", lambda: nc.gpsimd.memset(kiTd[0:64, 1, :], 0.0), w=("kiz1",))
                wi = sb(pa, "a_wi", [128, NB, 8], F32)
                absw = sb(pa, "a_absw", [128, NB, 8], F32)
                sgn = sb(pa, "a_sgn", [128, NB, 8], F32)
                T.op("pool", lambda: nc.gpsimd.memset(Vau[:, :, :, 64:128], 1.0), w=("Vau1",))
                norm_from_dram(G_ATT + 8 * l)
                if chk(2):
                    return
                with ExitStack() as ph:
                    cs = sb(ph, "a_cs", [128, 2 * T_], F32)
                    cosT = cs[:, 0:T_]
                    sinT = cs[:, T_:2 * T_]
                    T.dma("sp", "cs", [(cs[:], cst_d[:, 0:2 * T_])], w=("cos",))
                    E = alloc_epi(ph)
                    wv = winb[l].rearrange("(k p) n -> p k n", p=128)

                    def q_epi(tbase, dst, has_norm, gcol):
                        def epi(t_, ci, c0, n, pb, pk):
                            oi = E["oi"] % 2
                            E["oi"] += 1
                            o = E["o"][oi]
                            rope_epi(E, pb, pk, n, c0, 128, gcol, has_norm, [(o[:, 0:n], slice(0, 128))], (("e_o", oi),), cosT, sinT)
                            T.dma("sp", f"eo{oi}", [(dst[tbase + t_, :, c0:c0 + n], o[:, 0:n])], r=(("e_o", oi),))
                        return epi

                    def k_epi(t_, ci, c0, n, pb, pk):
                        g0, g1 = 2 * t_, 2 * t_ + 1
                        outs = [(KTd[0:64, 0, g0, c0:c0 + n], slice(0, 64)), (KTd[64:128, 1, g0, c0:c0 + n], slice(0, 64)),
                                (KTd[64:128, 1, g1, c0:c0 + n], slice(64, 128)), (KTd[0:64, 0, g1, c0:c0 + n], slice(64, 128))]
                        rope_epi(E, pb, pk, n, c0, 128, G_AK + 2 * l, True, outs, ("KTd",), cosT, sinT)

                    def ki_epi(t_, ci, c0, n, pb, pk):
                        outs = [(kiTd[0:64, 0, c0:c0 + n], slice(0, 64)), (kiTd[64:128, 1, c0:c0 + n], slice(0, 64))]
                        rope_epi(E, pb, pk, n, c0, 64, 0, False, outs, ("kiTd",), cosT, sinT)

                    feat_proj(wv, 0, 4, 128, q_epi(0, qT_d, True, G_AQ + 2 * l), f"win{l}")
                    if chk(21):
                        return
                    feat_proj(wv, 512, 4, 128, q_epi(4, qT_d, True, G_AQ + 2 * l), f"win{l}")
                    feat_proj(wv, 1024, 2, 128, k_epi, f"win{l}")
                    if chk(22):
                        return
                    feat_proj(wv, 1536, 4, 128, q_epi(0, qiT_d, False, 0), f"win{l}")
                    if chk(23):
                        return
                    feat_proj(wv, 2048, 1, 64, ki_epi, f"win{l}")
                    if chk(24):
                        return
                    i = wload(lambda s_: [(k8(s_[:, 0:8 * 264])[:, :, 0:256], wv[:, :, 1280:1536]),
                                          (k8(s_[:, 0:8 * 264])[:, :, 256:264], wv[:, :, 2112:2120])], f"win{l}")
                    wt = k8(WR[i][:, 0:8 * 264])
                    for b in range(NB):
                        pb = PS[b % 4]
                        pk = f"P{b % 4}"
                        c0 = CH[b // 4][0]
                        for kc in range(8):
                            mm(pb[:, 0:264], XT[:, kc, b * 128:(b + 1) * 128], wt[:, kc, :], kc == 0, kc == 7,
                               r=(("WR", i), ("XT", kc, c0)), w=(pk,))
                        cp("act" if b % 2 == 0 else "dve", Vau[:, b, :, 0:64], k8(pb[:, 0:256], 4), r=(pk,), w=("Vau",))
                        cp("dve", wi[:, b, :], pb[:, 256:264], r=(pk,), w=("wi",))
                    act(absw[:], wi[:], AF.Abs, r=("wi",), w=("absw",), scale=IDXSCALE)
                    ts("dve", sgn[:], wi[:], 0.0, 2.0, ALU.is_ge, ALU.mult, r=("wi",), w=("sgn",))
                    ts("dve", sgn[:], sgn[:], -1.0, None, ALU.add, None, r=("sgn",), w=("sgn",))
                    T.barrier()
                if chk(3):
                    return
                attn_A(KTd, Vau, kiTd, absw, sgn)

        def kv_phase2():
            norm_from_dram(G_KVN)
            kvv = kvwb.rearrange("(k p) n -> p k n", p=128)
            with ExitStack() as ph:
                fl = sb(ph, "k_fl", [16, T_], F32)
                with ExitStack() as ph2:
                    E = alloc_epi(ph2)
                    Vst = [sb(ph2, f"k_Vst{i}", [128, 8, 4, 128], BF16) for i in range(2)]
                    for i_ in range(2):
                        T.op("pool", lambda i_=i_: nc.gpsimd.memset(Vst[i_][:, :, :, 64:128], 1.0), w=(("Vst", i_),))
                    feat_proj(kvv, 0, 4, 128, headnorm_store_epi(E, gv[:, G_KVK:G_KVK + 1], KT_d, 0), "kvw")
                    feat_proj(kvv, 512, 4, 128, headnorm_store_epi(E, gv[:, G_KVK:G_KVK + 1], KT_d, 4), "kvw")
                    vi = 0
                    for half in range(2):
                        i = wload(lambda s_: [(k8(s_[:, :]), kvv[:, :, 1024 + 512 * half:1536 + 512 * half])], "kvw")
                        wt = k8(WR[i][:, :])
                        for b in range(NB):
                            pb = PS[b % 4]
                            pk = f"P{b % 4}"
                            ci = b // 4
                            bb = b % 4
                            c0, n = CH[ci]
                            if bb == 0:
                                vi += 1
                            st = Vst[vi % 2]
                            for kc in range(8):
                                mm(pb[:, :], XT[:, kc, b * 128:(b + 1) * 128], wt[:, kc, :], kc == 0, kc == 7,
                                   r=(("WR", i), ("XT", kc, c0)), w=(pk,))
                            cp("act" if b % 2 == 0 else "dve", st[:, :, bb, 0:64], k8(pb[:, :], 8), r=(pk,), w=(("Vst", vi % 2),))
                            if bb == n // 128 - 1:
                                nb_ = n // 128
                                T.dma("sp", f"vst{vi % 2}",
                                      [(Va_d[8 * half + hh, :, 4 * ci:4 * ci + nb_, :], st[:, hh, 0:nb_, :]) for hh in range(8)],
                                      r=(("Vst", vi % 2),))

                    def f_epi(t_, ci, c0, n, pb, pk):
                        cp("dve", fl[0:16, c0:c0 + n], pb[0:16, 0:n], r=(pk,), w=("fl",))
                    feat_proj(kvv, 2048, 1, 16, f_epi, "kvw")
                    T.barrier()
                on = sb(ph, "k_on", [16, T_], F32)
                cc = sb(ph, "k_c", [16, T_], F32)
                r1 = sb(ph, "k_r1", [16, T_], F32)
                qa = sb(ph, "k_qa", [16, 6, T_], BF16)
                ka = sb(ph, "k_ka", [16, 6, T_], BF16)
                T.op("pool", lambda: nc.gpsimd.memset(on[:, :], 1.0), w=("on",))
                T.op("pool", lambda: nc.gpsimd.memset(qa[:, 3:6, :], 1.0), w=("qa1",))
                T.op("pool", lambda: nc.gpsimd.memset(ka[:, 0:3, :], 1.0), w=("ka1",))
                act(r1[:, :], fl[:, :], AF.Exp, r=("fl",), w=("r1",), scale=-1.0, bias=nfb[0:16, 0:1])
                act(fl[:, :], r1[:, :], AF.Ln, r=("r1",), w=("fl",), bias=1.0)
                T.op("dve", lambda: nc.vector.tensor_tensor_scan(out=cc[:, :], data0=on[:, :], data1=fl[:, :], initial=0.0,
                                                                 op0=ALU.mult, op1=ALU.subtract), r=("on", "fl"), w=("cc",))
                cp("dve", qa[:, 0, :], cc[:, :], r=("cc",), w=("qa0",))
                tt("dve", r1[:, :], cc[:, :], qa[:, 0, :], ALU.subtract, r=("cc", "qa0"), w=("r1",))
                cp("dve", qa[:, 1, :], r1[:, :], r=("r1",), w=("qa1b",))
                tt("dve", fl[:, :], r1[:, :], qa[:, 1, :], ALU.subtract, r=("r1", "qa1b"), w=("fl",))
                cp("dve", qa[:, 2, :], fl[:, :], r=("fl",), w=("qa2",))
                ts("dve", ka[:, 3:6, :], qa[:, 0:3, :], -1.0, None, ALU.mult, None, r=("qa0", "qa1b", "qa2"), w=("ka2",))
                T.dma("sp", "caug", [(qaug_d[:, 64:70, :], qa[:, :, :]), (KT_d[:, 64:70, :], ka[:, :, :])],
                      r=("qa0", "qa1b", "qa2", "qa1", "ka1", "ka2"))
                T.barrier()

        def attn_B():
            with ExitStack() as ph:
                slots = [(sb(ph, f"b_qh{i}", [70, T_], BF16), sb(ph, f"b_kh{i}", [70, T_], BF16),
                          sb(ph, f"b_vh{i}", [128, NB, 128], BF16)) for i in range(3)]
                PT = [sb(ph, f"b_PT{i}", [128, 512], BF16) for i in range(6)]
                SBK = [0, 1, 2, 3, 4]
                scn = [0]
                rden = [sb(ph, f"b_rden{i}", [128, 512], F32) for i in range(2)]
                oc = 0
                for h in range(16):
                    sl = h % 3
                    qh, kh, vh = slots[sl]
                    hk = ("bh", sl)
                    T.dma("sp", f"bh{sl}", [(qh[:, :], qaug_d[h]), (kh[:, :], KT_d[h]), (vh[:, :, :], Va_d[h])], w=(hk,))
                    steps = []
                    for ci, (c0, n) in enumerate(CH):
                        O = PS[6 if oc % 2 == 0 else 5]
                        ok = "P6" if oc % 2 == 0 else "P5"
                        rd = rden[oc % 2]
                        rk = ("rden", oc % 2)
                        oc += 1
                        b0 = c0 // 128
                        jlast = b0 + n // 128 - 1
                        for j in range(jlast + 1):
                            rr = max(j - b0, 0)
                            col0 = c0 + rr * 128
                            ncols = c0 + n - col0
                            diag = j >= b0

                            def s_fn(j=j, col0=col0, ncols=ncols, diag=diag):
                                k_ = scn[0]
                                scn[0] += 1
                                Sp = PS[SBK[k_ % 5]]
                                sk = f"P{SBK[k_ % 5]}"
                                pt = PT[k_ % 6]
                                ptk = ("PT", k_ % 6)
                                mm(Sp[:, 0:ncols], kh[0:70, j * 128:(j + 1) * 128], qh[0:70, col0:col0 + ncols], True, not diag,
                                   r=(hk,), w=(sk,))
                                if diag:
                                    mm(Sp[:, 0:128], identb, triTb, False, True, r=(), w=(sk,))
                                act(pt[:, 0:ncols], Sp[:, 0:ncols], AF.Exp, r=(sk,), w=(ptk,))
                                return pt, ptk

                            def v_fn(st, j=j, col0=col0, ncols=ncols, c0=c0, n=n, O=O, ok=ok, rd=rd, rk=rk, jlast=jlast, h=h):
                                pt, ptk = st
                                mm(O[:, col0 - c0:col0 - c0 + ncols], vh[:, j, :], pt[:, 0:ncols], j == 0, j == jlast,
                                   r=(ptk, hk), w=(ok,))
                                if j == jlast:
                                    recip(rd[0:64, 0:n], O[64:128, 0:n], r=(ok,), w=(rk,))
                                    pbase = (h % 2) * 64
                                    tt("dve", XT[pbase:pbase + 64, h // 2, c0:c0 + n], O[0:64, 0:n], rd[0:64, 0:n], ALU.mult,
                                       r=(ok, rk), w=(("XT", h // 2, c0),))
                            steps.append((s_fn, v_fn))
                    run_pipe(steps, 3)
                T.barrier()

        def layer_B(j):
            norm_from_dram(G_ATT + 8 * (2 + j))
            with ExitStack() as ph:
                E = alloc_epi(ph)
                wv = bwqb[j].rearrange("(k p) n -> p k n", p=128)
                feat_proj(wv, 0, 4, 128, headnorm_store_epi(E, gq8[:, j:j + 1], qaug_d, 0), f"bwq{j}")
                feat_proj(wv, 512, 4, 128, headnorm_store_epi(E, gq8[:, j:j + 1], qaug_d, 4), f"bwq{j}")
                T.barrier()
            attn_B()

        def outproj_mlp(l, wo_d, s, last, wokey):
            with ExitStack() as ph:
                hTf = sb(ph, "m_hTf", [128, 8, T_], F32)
                for ci, (c0, n) in enumerate(CH):
                    T.dma("sp", "hload", [(hTf[:, :, c0:c0 + n], hT_d[:, :, c0:c0 + n].rearrange("k p n -> p k n"))],
                          w=tuple(("hTf", m, c0) for m in range(8)))
                wov = wo_d.rearrange("(k p) n -> p k n", p=128)

                def o_epi(tbase):
                    def epi(t_, ci, c0, n, pb, pk):
                        m = tbase + t_
                        tt("dve", hTf[:, m, c0:c0 + n], hTf[:, m, c0:c0 + n], pb[:, 0:n], ALU.add, r=(pk, ("hTf", m, c0)),
                           w=(("hTf", m, c0),))
                    return epi
                feat_proj(wov, 0, 4, 128, o_epi(0), wokey)
                feat_proj(wov, 512, 4, 128, o_epi(4), wokey)
                with ExitStack() as ph2:
                    sq = sb(ph2, "n_sq", [128, 8, 512], BF16)
                    std = sb(ph2, "n_std", [128, 512], F32)
                    rstd = sb(ph2, "n_rstd", [128, 512], F32)
                    for ci, (c0, n) in enumerate(CH):
                        norm_cols(lambda kc, c0=c0, n=n: hTf[:, kc, c0:c0 + n], tuple(("hTf", m, c0) for m in range(8)),
                                  G_MLP + 8 * l, c0, n, (sq, std, rstd))
                    T.barrier()
                with ExitStack() as ph2:
                    aT = [sb(ph2, f"m_aT{i}", [128, 4, T_], BF16) for i in range(2)]
                    rt = [sb(ph2, f"m_rt{i}", [128, 512], F32) for i in range(2)]
                    w1v = w1b[l].rearrange("(k p) n -> p k n", p=128)
                    w2v = w2b[l].rearrange("(f p) n -> p f n", p=128)
                    ri = 0
                    pc = 0
                    yc = 0
                    for fg in range(8):
                        i1 = wload(lambda s_: [(k8(s_[:, :]), w1v[:, :, fg * 512:(fg + 1) * 512])], f"w1_{l}")
                        w1t = k8(WR[i1][:, :])
                        a = aT[fg % 2]
                        for f in range(4):
                            for ci, (c0, n) in enumerate(CH):
                                pidx = pc % 4
                                pc += 1
                                pb = PS[pidx]
                                pk = f"P{pidx}"
                                for kc in range(8):
                                    mm(pb[:, 0:n], w1t[:, kc, f * 128:(f + 1) * 128], XT[:, kc, c0:c0 + n], kc == 0, kc == 7,
                                       r=(("WR", i1), ("XT", kc, c0)), w=(pk,))
                                r_ = rt[ri % 2]
                                rk = ("rt", ri % 2)
                                ri += 1
                                act(r_[:, 0:n], pb[:, 0:n], AF.Relu, r=(pk,), w=(rk,))
                                tt("pool", a[:, f, c0:c0 + n], r_[:, 0:n], r_[:, 0:n], ALU.mult, r=(rk,), w=(("aT", fg % 2, f, c0),))
                        i2 = wload(lambda s_: [(k8(s_[:, :], 4), w2v[:, fg * 4:(fg + 1) * 4, :])], f"w2_{l}")
                        w2t = k8(WR[i2][:, :], 4)
                        for m in range(8):
                            for ci, (c0, n) in enumerate(CH):
                                pidx = 4 + yc % 4
                                yc += 1
                                pb = PS[pidx]
                                pk = f"P{pidx}"
                                for f in range(4):
                                    mm(pb[:, 0:n], w2t[:, f, m * 128:(m + 1) * 128], a[:, f, c0:c0 + n], f == 0, f == 3,
                                       r=(("WR", i2), ("aT", fg % 2, f, c0)), w=(pk,))
                                tt("dve", hTf[:, m, c0:c0 + n], hTf[:, m, c0:c0 + n], pb[:, 0:n], ALU.add,
                                   r=(pk, ("hTf", m, c0)), w=(("hTf", m, c0),))
                    T.barrier()
                if not last:
                    for ci, (c0, n) in enumerate(CH):
                        T.dma("sp", "hstore", [(hT_d[:, :, c0:c0 + n].rearrange("k p n -> p k n"), hTf[:, :, c0:c0 + n])])
                    if dbg_d is not None and s == 0:
                        T.dma("sp", "dbgst", [(dbg_d[:, :, :].rearrange("k p n -> p k n"), hTf[:, :, :])])
                else:
                    if dbg_d is not None and s == 0:
                        T.dma("sp", "dbgst", [(dbg_d[:, :, :].rearrange("k p n -> p k n"), hTf[:, :, :])])
                    with ExitStack() as ph2:
                        og = [sb(ph2, f"o_st{i}", [128, D], F32) for i in range(2)]
                        for b in range(NB):
                            o = og[b % 2]
                            okk = ("ost", b % 2)
                            for half in range(2):
                                pidx = (2 * b + half) % 4
                                pb = PS[pidx]
                                pk = f"P{pidx}"
                                for jq in range(4):
                                    kc = half * 4 + jq
                                    T.op("pe", lambda pb=pb, jq=jq, kc=kc, b=b: nc.tensor.transpose(
                                        out=pb[:, jq * 128:(jq + 1) * 128], in_=hTf[:, kc, b * 128:(b + 1) * 128], identity=identf),
                                        r=(), w=(pk,), sig=(jq == 3))
                                cp("act" if half == 0 else "dve", o[:, half * 512:(half + 1) * 512], pb[:, :], r=(pk,), w=(okk,))
                            if b == 0:
                                T.dma("sp", f"ost{b % 2}", [(out_d[s, 0:112, :], o[16:128, :])], r=(okk,))
                            elif b < 16:
                                T.dma("sp", f"ost{b % 2}", [(out_d[s, 128 * b - 16:128 * b + 112, :], o[:, :])], r=(okk,))
                            else:
                                T.dma("sp", f"ost{b % 2}", [(out_d[s, 2032:2048, :], o[0:16, :])], r=(okk,))
                        T.barrier()
                T.barrier()

        chk(0)
        for s in range(n_seq):
            if done[0]:
                break
            hT_d = hT_all[s]
            input_phase(s)
        chk(1)
        for s in range(n_seq):
            if done[0]:
                break
            hT_d = hT_all[s]
            for l in range(n_layers):
                last = l == n_layers - 1
                if l < 2:
                    layer_A(l)
                    if done[0] or chk(4):
                        break
                    outproj_mlp(l, woutb[l], s, last, f"wout{l}")
                else:
                    if l == 2:
                        kv_phase2()
                    layer_B(l - 2)
                    outproj_mlp(l, bwoutb[l - 2], s, last, f"bwout{l - 2}")
        T.finish()
        stats = (T.nins, T.nwait, dict(T.cnt))
    return nc, stats


def host_consts():
    inv = (1.0 / (np.float32(10000.0) ** (np.arange(0, 64, 2, dtype=np.float32) / np.float32(64)))).astype(np.float32)
    ang = (np.arange(T_, dtype=np.float32)[:, None] * inv[None, :]).astype(np.float32)
    cos = np.cos(ang).astype(np.float32)
    sin = np.sin(ang).astype(np.float32)
    cst = np.zeros((128, C_END), np.float32)
    p = np.arange(128)
    d = p % 64
    cst[:, C_COS:C_COS + T_] = cos[:, d % 32].T
    sg = np.where(d < 32, -1.0, 1.0).astype(np.float32)
    cst[:, C_SIN:C_SIN + T_] = sin[:, d % 32].T * sg[:, None]
    cst[:, C_ID:C_ID + 128] = np.eye(128, dtype=np.float32)
    cst[:, C_BONES:C_BONES + 128] = (p[:, None] // 64 == p[None, :] // 64).astype(np.float32)
    cst[:, C_PSW:C_PSW + 128] = (p[:, None] == (p[None, :] ^ 32)).astype(np.float32)
    cst[:, C_TRIQS:C_TRIQS + 128] = np.where(p[None, :] > p[:, None], -1e30, 0.0).astype(np.float32)
    cst[:, C_TRIT:C_TRIT + 128] = np.where(p[:, None] > p[None, :], NEG, 0.0).astype(np.float32)
    cst[:, C_POW:C_POW + 32] = (0.5 ** np.arange(1, 33, dtype=np.float64)).astype(np.float32)[None, :]
    cst[:, C_ONES:C_ONES + 128] = 1.0
    return cst


def host_gv(attn_norm, mlp_norm, kv_norm, a_q_gain, a_k_gain, kv_k_gain, b_q_gain, kv_f_bias):
    gv = np.zeros((128, G_END), np.float32)
    p = np.arange(128)
    d = p % 64
    for l in range(4):
        gv[:, G_ATT + 8 * l:G_ATT + 8 * l + 8] = np.asarray(attn_norm[l]).reshape(8, 128).T
        gv[:, G_MLP + 8 * l:G_MLP + 8 * l + 8] = np.asarray(mlp_norm[l]).reshape(8, 128).T
    gv[:, G_KVN:G_KVN + 8] = np.asarray(kv_norm).reshape(8, 128).T
    for l in range(2):
        gv[:, G_AQ + 2 * l] = np.asarray(a_q_gain[l])[d]
        gv[:, G_AQ + 2 * l + 1] = np.asarray(a_q_gain[l])[d ^ 32]
        gv[:, G_AK + 2 * l] = np.asarray(a_k_gain[l])[d]
        gv[:, G_AK + 2 * l + 1] = np.asarray(a_k_gain[l])[d ^ 32]
        gv[:, G_BQ + l] = np.asarray(b_q_gain[l])[d]
    gv[:, G_KVK] = np.asarray(kv_k_gain)[d]
    gv[0:16, G_FB] = np.asarray(kv_f_bias)
    return gv


_CACHE = {}


def kernel(x, meta_tokens, attn_norm, mlp_norm, mlp_w1, mlp_w2, a_w_in, a_q_gain, a_k_gain, a_w_out,
           kv_norm, kv_w, kv_f_bias, kv_k_gain, b_w_q, b_q_gain, b_w_out, _n_layers=4, _dbg=False, _n_seq=2, _trace=False, _stop=None, _cores=8):
    f = lambda a: np.ascontiguousarray(np.asarray(a, dtype=np.float32))
    x = f(x)
    key = (_n_layers, _dbg, _n_seq, _stop)
    if key not in _CACHE:
        _CACHE[key] = build(_n_layers, _dbg, _n_seq, _stop)
    nc, stats = _CACHE[key]
    shared = {
        "meta": f(meta_tokens), "mlp_w1": f(mlp_w1), "mlp_w2": f(mlp_w2), "a_w_in": f(a_w_in), "a_w_out": f(a_w_out),
        "kv_w": f(kv_w), "b_w_q": f(b_w_q), "b_w_out": f(b_w_out), "cst": host_consts(),
        "gv": host_gv(f(attn_norm), f(mlp_norm), f(kv_norm), f(a_q_gain), f(a_k_gain), f(kv_k_gain), f(b_q_gain), f(kv_f_bias)),
    }
    in_maps = []
    for c in range(_cores):
        m = dict(shared)
        m["x"] = np.ascontiguousarray(x[2 * c:2 * c + 2])
        in_maps.append(m)
    if _trace:
        res = run_bass_kernel_spmd(nc, in_maps, core_ids=list(range(_cores)), trace=True)
        print('exec_time_ns', res.exec_time_ns)
    else:
        res = run_bass_kernel_spmd(nc, in_maps, core_ids=list(range(_cores)))
    out = np.concatenate([np.asarray(r["out"]) for r in res.results], axis=0).astype(np.float32)
    if _dbg:
        return out, [np.asarray(r["dbg"]) for r in res.results]
    return out
```
